# Optimizing a Trainium2 kernel written in Bass

```python
import jax, jax.numpy as jnp
from jax import lax
import numpy as np

D_MODEL = 1024
BATCH = 1
SEQ = 16384
DEPTH = 2

N_EVEN = (DEPTH + 1) // 2
N_ODD = DEPTH // 2
N_SUBLAYERS = 3
D_FF = 2816
EPS = 1e-6

A_WIDTH = D_MODEL // 2
A_GROUPS = 8
A_CONV = 31
B_WIDTH = D_MODEL // 2
B_CONV = 3
EVEN_IN = 2 * A_WIDTH + 3 * B_WIDTH
EVEN_MIX = A_WIDTH + B_WIDTH

POOL_WINDOWS = (2, 4, 8, 16)
C_GROUPS = len(POOL_WINDOWS)
C_WIDTH = D_MODEL // 2
C_GROUP_DIM = C_WIDTH // C_GROUPS
D_HEADS = 8
HEAD_DIM = 64
D_WIDTH = D_HEADS * HEAD_DIM
ODD_IN = C_WIDTH + 3 * D_WIDTH
ODD_MIX = C_WIDTH + D_WIDTH
MOBA_BLOCK = 256
MOBA_TOPK = 3
Q_CHUNK = 128

kernel_name = "hybrid_conv_pool_moba_macaron_adaln"


def rms_norm(x, g):
    xf = x.astype(jnp.float32)
    y = xf * lax.rsqrt(jnp.mean(xf * xf, axis=-1, keepdims=True) + EPS)
    return (y * g.astype(jnp.float32)).astype(x.dtype)


def modulate(h, shift, scale):
    return h * (1.0 + scale[:, None, :]) + shift[:, None, :]


def group_layer_norm(u, n_groups, g, b):
    bn, s, ch = u.shape
    uf = u.astype(jnp.float32).reshape(bn, s, n_groups, ch // n_groups)
    mu = jnp.mean(uf, axis=-1, keepdims=True)
    var = jnp.mean(jnp.square(uf - mu), axis=-1, keepdims=True)
    y = ((uf - mu) * lax.rsqrt(var + EPS)).reshape(bn, s, ch)
    return (y * g + b).astype(u.dtype)


def causal_depthwise_conv(u, w):
    k, ch = w.shape
    return lax.conv_general_dilated(
        u, w[:, None, :], window_strides=(1,), padding=[(k - 1, 0)],
        dimension_numbers=("NWC", "WIO", "NWC"), feature_group_count=ch)


def swiglu(h, w_gate, w_up, w_down):
    return (jax.nn.silu(h @ w_gate) * (h @ w_up)) @ w_down


def conv_mixers(h, w_in, conv_a_w, conv_a_b, ln_a_g, ln_a_b, conv_b_w, w_out):
    z = h @ w_in
    a_val, a_gate, b_gate, c_gate, b_val = jnp.split(
        z, [A_WIDTH, 2 * A_WIDTH, 2 * A_WIDTH + B_WIDTH, 2 * A_WIDTH + 2 * B_WIDTH], axis=-1)
    a = a_val * jax.nn.sigmoid(a_gate)
    a = causal_depthwise_conv(a, conv_a_w) + conv_a_b
    a = jax.nn.silu(group_layer_norm(a, A_GROUPS, ln_a_g, ln_a_b))
    bb = b_gate * causal_depthwise_conv(c_gate * b_val, conv_b_w)
    return jnp.concatenate([a, bb], axis=-1) @ w_out


def multiscale_pool(u, pool_w, pool_b, pool_scale):
    bn, s, _ = u.shape
    ug = u.reshape(bn, s, C_GROUPS, C_GROUP_DIM).astype(jnp.float32)
    cs0 = jnp.pad(jnp.cumsum(ug, axis=1), ((0, 0), (1, 0), (0, 0), (0, 0)))
    t1 = jnp.arange(1, s + 1, dtype=jnp.float32)
    means = []
    for g, w in enumerate(POOL_WINDOWS):
        upper = cs0[:, 1:, g]
        lower = jnp.pad(cs0[:, :s + 1 - w, g], ((0, 0), (w - 1, 0), (0, 0)))
        means.append((upper - lower) / jnp.minimum(t1, w)[None, :, None])
    pooled = (jnp.stack(means, axis=2) - ug).astype(u.dtype)
    mixed = jnp.einsum("bsgc,gcd->bsgd", pooled, pool_w) + pool_b
    return mixed.reshape(bn, s, C_WIDTH) * pool_scale


def moba_attention(q, k, v):
    bn, s, _ = q.shape
    s_pad = -(-s // MOBA_BLOCK) * MOBA_BLOCK
    nb = s_pad // MOBA_BLOCK
    topk = min(MOBA_TOPK, nb)

    def to_heads(t):
        return t.reshape(bn, s, D_HEADS, HEAD_DIM).transpose(0, 2, 1, 3)

    pad = ((0, 0), (0, 0), (0, s_pad - s), (0, 0))
    qh = to_heads(q) * (HEAD_DIM ** -0.5)
    kh = jnp.pad(to_heads(k), pad)
    vh = jnp.pad(to_heads(v), pad)
    k_blocks = kh.reshape(bn, D_HEADS, nb, MOBA_BLOCK, HEAD_DIM)
    v_blocks = vh.reshape(bn, D_HEADS, nb, MOBA_BLOCK, HEAD_DIM)
    k_mean = jnp.mean(k_blocks.astype(jnp.float32), axis=3)
    slopes = jnp.asarray(2.0 ** (-8.0 * np.arange(1, D_HEADS + 1) / D_HEADS), dtype=jnp.float32)
    b_idx = jnp.arange(bn)[:, None, None, None]
    h_idx = jnp.arange(D_HEADS)[None, :, None, None]
    offs = jnp.arange(MOBA_BLOCK)
    blk_ids = jnp.arange(nb)
    slot_ids = jnp.arange(topk)

    def chunk(ci):
        q0 = ci * Q_CHUNK
        own = q0 // MOBA_BLOCK
        qc = lax.dynamic_slice_in_dim(qh, q0, Q_CHUNK, axis=2)
        qpos = q0 + jnp.arange(Q_CHUNK)
        gate = jnp.einsum("bhqd,bhnd->bhqn", qc.astype(jnp.float32), k_mean)
        gate = jnp.where(blk_ids < own, gate, -jnp.inf)
        _, sel = lax.top_k(gate, topk)
        sel_valid = slot_ids < own
        k_sel = k_blocks[b_idx, h_idx, sel]
        v_sel = v_blocks[b_idx, h_idx, sel]
        s_sel = jnp.einsum("bhqd,bhqjkd->bhqjk", qc, k_sel).astype(jnp.float32)
        kpos_sel = sel[..., None] * MOBA_BLOCK + offs
        s_sel = s_sel - slopes[:, None, None, None] * (qpos[:, None, None] - kpos_sel)
        s_sel = jnp.where(sel_valid[:, None], s_sel, -jnp.inf)
        s_sel = s_sel.reshape(bn, D_HEADS, Q_CHUNK, topk * MOBA_BLOCK)
        k_own = lax.dynamic_slice_in_dim(kh, own * MOBA_BLOCK, MOBA_BLOCK, axis=2)
        v_own = lax.dynamic_slice_in_dim(vh, own * MOBA_BLOCK, MOBA_BLOCK, axis=2)
        dist = qpos[:, None] - (own * MOBA_BLOCK + offs)[None, :]
        s_own = jnp.einsum("bhqd,bhkd->bhqk", qc, k_own).astype(jnp.float32)
        s_own = jnp.where(dist >= 0, s_own - slopes[:, None, None] * dist, -jnp.inf)
        p = jax.nn.softmax(jnp.concatenate([s_sel, s_own], axis=-1), axis=-1).astype(v.dtype)
        p_sel, p_own = jnp.split(p, [topk * MOBA_BLOCK], axis=-1)
        p_sel = p_sel.reshape(bn, D_HEADS, Q_CHUNK, topk, MOBA_BLOCK)
        return (jnp.einsum("bhqjk,bhqjkd->bhqd", p_sel, v_sel)
                + jnp.einsum("bhqk,bhkd->bhqd", p_own, v_own))

    out = lax.map(chunk, jnp.arange(s // Q_CHUNK))
    return out.transpose(1, 0, 3, 2, 4).reshape(bn, s, D_WIDTH)


def pool_moba_mixers(h, w_in, pool_w, pool_b, pool_scale, w_out):
    z = h @ w_in
    u, q, k, v = jnp.split(z, [C_WIDTH, C_WIDTH + D_WIDTH, C_WIDTH + 2 * D_WIDTH], axis=-1)
    pooled = multiscale_pool(u, pool_w, pool_b, pool_scale)
    att = moba_attention(q, k, v)
    return jnp.concatenate([pooled, att], axis=-1) @ w_out


def setup_inputs(seed: int = 0) -> dict:
    key = jax.random.key(seed)
    ks = jax.random.split(key, 24)
    f32 = jnp.float32

    def nrm(k, shape, scale):
        return jax.random.normal(k, shape, dtype=f32) * scale

    return {
        "x": nrm(ks[0], (BATCH, SEQ, D_MODEL), 1.0),
        "c": nrm(ks[1], (BATCH, D_MODEL), 1.0),
        "ada_w": nrm(ks[2], (DEPTH, D_MODEL, N_SUBLAYERS * 3 * D_MODEL), D_MODEL ** -0.5),
        "ada_b": nrm(ks[3], (DEPTH, N_SUBLAYERS * 3 * D_MODEL), 0.02),
        "ffn_norm": 1.0 + nrm(ks[4], (DEPTH, 2, D_MODEL), 0.05),
        "ffn_w_gate": nrm(ks[5], (DEPTH, 2, D_MODEL, D_FF), D_MODEL ** -0.5),
        "ffn_w_up": nrm(ks[6], (DEPTH, 2, D_MODEL, D_FF), D_MODEL ** -0.5),
        "ffn_w_down": nrm(ks[7], (DEPTH, 2, D_FF, D_MODEL), D_FF ** -0.5),
        "mix_norm": 1.0 + nrm(ks[8], (DEPTH, D_MODEL), 0.05),
        "conv_w_in": nrm(ks[9], (N_EVEN, D_MODEL, EVEN_IN), D_MODEL ** -0.5),
        "conv_a_w": nrm(ks[10], (N_EVEN, A_CONV, A_WIDTH), A_CONV ** -0.5),
        "conv_a_b": nrm(ks[11], (N_EVEN, A_WIDTH), 0.02),
        "conv_a_ln_g": 1.0 + nrm(ks[12], (N_EVEN, A_WIDTH), 0.05),
        "conv_a_ln_b": nrm(ks[13], (N_EVEN, A_WIDTH), 0.02),
        "conv_b_w": nrm(ks[14], (N_EVEN, B_CONV, B_WIDTH), B_CONV ** -0.5),
        "conv_w_out": nrm(ks[15], (N_EVEN, EVEN_MIX, D_MODEL), EVEN_MIX ** -0.5),
        "pm_w_in": nrm(ks[16], (N_ODD, D_MODEL, ODD_IN), D_MODEL ** -0.5),
        "pool_w": nrm(ks[17], (N_ODD, C_GROUPS, C_GROUP_DIM, C_GROUP_DIM), C_GROUP_DIM ** -0.5),
        "pool_b": nrm(ks[18], (N_ODD, C_GROUPS, C_GROUP_DIM), 0.02),
        "pool_scale": 1.0 + nrm(ks[19], (N_ODD, C_WIDTH), 0.1),
        "pm_w_out": nrm(ks[20], (N_ODD, ODD_MIX, D_MODEL), ODD_MIX ** -0.5),
        "final_norm": 1.0 + nrm(ks[21], (D_MODEL,), 0.05),
    }


def reference(x, c, ada_w, ada_b, ffn_norm, ffn_w_gate, ffn_w_up, ffn_w_down, mix_norm,
              conv_w_in, conv_a_w, conv_a_b, conv_a_ln_g, conv_a_ln_b, conv_b_w, conv_w_out,
              pm_w_in, pool_w, pool_b, pool_scale, pm_w_out, final_norm):
    bn = x.shape[0]
    cond = jax.nn.silu(c)
    for i in range(DEPTH):
        mod = (cond @ ada_w[i] + ada_b[i]).reshape(bn, N_SUBLAYERS, 3, D_MODEL)
        h = modulate(rms_norm(x, ffn_norm[i, 0]), mod[:, 0, 0], mod[:, 0, 1])
        x = x + 0.5 * mod[:, 0, 2][:, None, :] * swiglu(
            h, ffn_w_gate[i, 0], ffn_w_up[i, 0], ffn_w_down[i, 0])
        h = modulate(rms_norm(x, mix_norm[i]), mod[:, 1, 0], mod[:, 1, 1])
        if i % 2 == 0:
            e = i // 2
            y = conv_mixers(h, conv_w_in[e], conv_a_w[e], conv_a_b[e], conv_a_ln_g[e],
                            conv_a_ln_b[e], conv_b_w[e], conv_w_out[e])
        else:
            o = i // 2
            y = pool_moba_mixers(h, pm_w_in[o], pool_w[o], pool_b[o], pool_scale[o], pm_w_out[o])
        x = x + mod[:, 1, 2][:, None, :] * y
        h = modulate(rms_norm(x, ffn_norm[i, 1]), mod[:, 2, 0], mod[:, 2, 1])
        x = x + 0.5 * mod[:, 2, 2][:, None, :] * swiglu(
            h, ffn_w_gate[i, 1], ffn_w_up[i, 1], ffn_w_down[i, 1])
    return rms_norm(x, final_norm)
```

```python
import numpy as np
from contextlib import ExitStack

import concourse.bass as bass
import concourse.mybir as mybir
from concourse.bass_utils import run_bass_kernel_spmd

F32 = mybir.dt.float32
BF16 = mybir.dt.bfloat16
AF = mybir.ActivationFunctionType
ALU = mybir.AluOpType
AX = mybir.AxisListType

NCORES = 8
D = 1024
SEQ = 16384
TOK = SEQ // NCORES
HALO = 64
TT = TOK + HALO
DFF = 2816
NFC = DFF // 128
EPS = 1.0000001e-6
NEG = -30000.0

TILES_H = [(0, 64), (64, 512), (576, 512), (1088, 512), (1600, 512)]
TILES = TILES_H[1:]


class Sem:
    def __init__(self, name):
        self.name = name
        self.h = None
        self.count = 0


class Buf:
    __slots__ = ("name", "w", "r", "dsem")

    def __init__(self, name=""):
        self.name = name
        self.dsem = None
        self.w = None
        self.r = {}


class Eng:
    def __init__(self, name):
        self.name = name
        self.sem = Sem("e_" + name)
        self.ops = []
        self.known = {}


class Prog:
    def __init__(self):
        self.E = {n: Eng(n) for n in ("pe", "act", "dve", "pool", "sp")}
        self.sems = [e.sem for e in self.E.values()]
        self.frontier = {}
        self.scoped = []

    def buf(self, name=""):
        b = Buf(name)
        b.r = dict(self.frontier)
        return b

    def advance_frontier(self):
        for e in self.E.values():
            if e.sem.count:
                self.frontier[e.sem] = e.sem.count
        for sm in self.scoped:
            if sm.count:
                self.frontier[sm] = sm.count

    def new_sem(self, name):
        s = Sem(name)
        self.sems.append(s)
        return s

    def _wait(self, e, s, v):
        if e.known.get(s, 0) >= v:
            return
        e.known[s] = v
        e.ops.append(("wait", s, v))

    def _deps(self, e, reads, writes):
        for b in reads:
            if b.w is not None:
                self._wait(e, *b.w)
        for b in writes:
            if b.w is not None:
                self._wait(e, *b.w)
            for s, v in b.r.items():
                self._wait(e, s, v)

    def _stamp(self, stamp, reads, writes):
        s, v = stamp
        for b in reads:
            if b.r.get(s, 0) < v:
                b.r[s] = v
        for b in writes:
            b.w = stamp
            b.r = {}

    def op(self, eng, fn, reads=(), writes=()):
        e = self.E[eng]
        if eng == "pe":
            known_self = e.known.get(e.sem, 0)
            e.known[e.sem] = 1 << 60
            self._deps(e, reads, writes)
            e.known[e.sem] = known_self
        else:
            self._deps(e, reads, writes)
        e.sem.count += 1
        e.ops.append(("op", fn, e.sem, 1))
        self._stamp((e.sem, e.sem.count), reads, writes)

    def dma(self, q, sem, out, in_, reads=(), writes=()):
        e = self.E[q]
        if sem is None:
            sem = self.bufsem(writes[0])
        self._deps(e, reads, writes)
        sem.count += 16
        e.ops.append(("op", (lambda g, o=out, i=in_: g.dma_start(out=o, in_=i)), sem, 16))
        self._stamp((sem, sem.count), reads, writes)

    def bufsem(self, b):
        if b.dsem is None:
            b.dsem = self.new_sem("d%d" % len(self.sems))
            self.scoped.append(b.dsem)
        return b.dsem

    def custom(self, q, sem, inc, fn, reads=(), writes=()):
        e = self.E[q]
        if sem is None:
            sem = self.bufsem(writes[0])
        self._deps(e, reads, writes)
        sem.count += inc
        e.ops.append(("op", fn, sem, inc))
        self._stamp((sem, sem.count), reads, writes)

    def emit(self, nc, final_waits):
        with ExitStack() as st:
            for s in self.sems:
                s.h = st.enter_context(nc.semaphore(s.name))
            block = st.enter_context(nc.Block())
            handles = {"pe": block.tensor, "act": block.scalar, "dve": block.vector,
                       "pool": block.gpsimd, "sp": block.sync}
            for name, e in self.E.items():
                ops = list(e.ops)
                if name == "sp":
                    for s, v in final_waits:
                        ops.append(("wait", s, v))

                def body(g, ops=ops):
                    for o in ops:
                        if o[0] == "wait":
                            g.wait_ge(o[1].h, o[2])
                        else:
                            ins = o[1](g)
                            ins.then_inc(o[2].h, o[3])
                handles[name](body)


class Arena:
    def __init__(self, ap, nwords, prog=None):
        self.prog = prog
        self.ap = ap
        self.n = nwords
        self.top = 0
        self.marks = []

    def push(self):
        self.marks.append(self.top)

    def pop(self):
        self.top = self.marks.pop()
        if self.prog is not None:
            self.prog.advance_frontier()

    def f32(self, n):
        a = self.ap[:, self.top:self.top + n]
        self.top += (n + 7) // 8 * 8
        assert self.top <= self.n, ("SBUF arena overflow", self.top, self.n)
        return a

    def bf16(self, n):
        w = (n + 1) // 2
        return self.f32(w).bitcast(BF16)[:, 0:n]


class Builder:
    def __init__(self, stop_after=None, gather=False):
        self.stop_after = stop_after
        self.gather = gather
        self.b_nodep = Buf("nodep")
        self.nc = bass.Bass("TRN2", target_bir_lowering=False)
        self.P = Prog()
        self.ins = {}
        self.wtasks = []
        self.wnext = 0
        self.wissued = 0
        self.wreleased = 0

    def din(self, name, shape, dt=F32):
        t = self.nc.dram_tensor(name, list(shape), dt, kind="ExternalInput").ap()
        self.ins[name] = t
        return t

    def win(self, name, rows, cols):
        nc, P = self.nc, self.P
        if not self.gather:
            return self.din(name, [rows, cols]), self.b_nodep
        part = self.din(name, [rows // NCORES, cols])
        full = nc.dram_tensor(name + "_full", [rows, cols], F32, kind="Internal").ap()
        parti = nc.dram_tensor(name + "_pi", [rows // NCORES, cols], F32, kind="Internal").ap()
        b = Buf(name)
        bp = Buf(name + "p")
        P.dma("sp", None, parti, part, writes=(bp,))
        sem = P.new_sem("g_" + name)
        P.custom("pool", sem, 1,
                 lambda g: g.collective_compute("AllGather", ALU.bypass, replica_groups=[list(range(NCORES))],
                                                ins=[parti[:, :]], outs=[full[:, :]]),
                 reads=(bp,), writes=(b,))
        return full, b

    def act(self, out, in_, func, reads, writes, bias=None, scale=None):
        kw = {}
        if bias is not None:
            kw["bias"] = bias
        if scale is not None:
            kw["scale"] = scale
        self.P.op("act", lambda g: g.activation(out=out, in_=in_, func=func, **kw), reads, writes)

    def mm(self, mms, reads, writes):
        def fn(g, mms=mms):
            ins = None
            for (o, l, r, s0, s1) in mms:
                ins = g.matmul(o, l, r, start=s0, stop=s1)
            return ins
        self.P.op("pe", fn, reads, writes)

    def wplan(self, src_ap, view_fn, dep):
        self.wtasks.append((src_ap, view_fn, dep))

    def w_pump(self):
        while self.wissued < len(self.wtasks) and self.wissued < self.wreleased + self.NS:
            i = self.wissued
            src, vf, dep = self.wtasks[i]
            slot = i % self.NS
            if isinstance(src, list):
                for (s_ap, sub) in src:
                    self.P.dma("pool", self.wsem[slot], sub(vf(self.wslot[slot])), s_ap, reads=(dep,),
                               writes=(self.wbuf[slot],))
            else:
                self.P.dma("pool", self.wsem[slot], vf(self.wslot[slot]), src, reads=(dep,),
                           writes=(self.wbuf[slot],))
            self.wissued += 1

    def w_get(self):
        i = self.wnext
        assert i < self.wissued, "weight task not issued (ring too small for this group)"
        self.wnext += 1
        src, vf, dep = self.wtasks[i]
        slot = i % self.NS
        return vf(self.wslot[slot]), self.wbuf[slot]

    def w_release(self, k):
        self.wreleased += k
        self.w_pump()

    def build(self):
        nc, P = self.nc, self.P
        st = ExitStack()
        self.st = st
        xT = self.din("xT", [D, TT])
        cT = self.din("cT", [128, 8])
        adaw = self.din("adaw", [18, D, 128])
        adab = self.din("adab", [128, 18])
        normg = self.din("normg", [128, 7, 8])
        wg, b_wg = self.win("ffn_wg", 4 * D, DFF)
        wu, b_wu = self.win("ffn_wu", 4 * D, DFF)
        wd, b_wd = self.win("ffn_wd", 4 * DFF, D)
        self.wdeps = (b_wg, b_wu, b_wd)
        self.w_cin = self.win("conv_w_in", D, 2560)
        self.w_cout = self.win("conv_w_out", D, D)
        self.cvec_d = self.din("cvec", [128, 4 * 37])
        self.w_pin = self.win("pm_w_in", D, 2048)
        self.w_pout = self.win("pm_w_out", D, D)
        self.poolw_d = self.din("pool_w", [4, 128, 128])
        self.pvec_d = self.din("pvec", [128, 8])
        self.pcorr_d = self.din("pcorr", [128, 64])
        self.khot_d = self.din("khot", [64, SEQ], BF16)
        self.aconst_d = self.din("aconst", [128, 4])
        self.drev_d = self.din("drev", [128, 127])
        self.tri_d = self.din("tri", [128, 512], BF16)
        self.ident_d = self.din("ident", [128, 128], BF16)
        self.hmask_d = self.din("hmask", [128, 64])
        outT = nc.dram_tensor("outT", [D, TOK], F32, kind="ExternalOutput").ap()
        mod_send = nc.dram_tensor("mod_send", [128, 18], F32, kind="Internal").ap()
        mod_all = nc.dram_tensor("mod_all", [NCORES * 128, 18], F32, kind="Internal").ap()

        NW = 212000 // 4
        arena_t = st.enter_context(nc.sbuf_tensor("arena", [128, NW], F32))
        A = Arena(arena_t, NW, P)
        self.A = A
        ps = st.enter_context(nc.psum_tensor("ps", [128, 7, 512], F32))
        self.psT = st.enter_context(nc.psum_tensor("psT", [128, 1024], BF16))
        self.psTb = Buf("psT")
        self.ps = ps
        self.psb = [Buf("ps%d" % i) for i in range(8)]

        xs = A.f32(8 * TT).rearrange("p (c t) -> p c t", c=8)
        self.xs = xs
        self.xb = {t: Buf("x%d" % t[0]) for t in TILES_H}
        modT = A.f32(144)
        vecA = A.f32(48)
        vecG = A.f32(48)
        ng = A.f32(56).rearrange("p (a c) -> p a c", a=7)
        cond = A.f32(8)
        epsc = A.f32(8)
        ones_bf = A.bf16(128)
        self.modT, self.vecA, self.vecG, self.ng = modT, vecA, vecG, ng
        self.epsc, self.ones_bf = epsc, ones_bf
        b_const = Buf("const")
        self.b_const = b_const
        b_mod = Buf("mod")
        self.b_mod = b_mod
        self.n_sq = [A.bf16(8 * 256).rearrange("p (c t) -> p c t", c=8) for _ in range(2)]
        self.n_tmp = [A.f32(8 * 256).rearrange("p (c t) -> p c t", c=8) for _ in range(1)]
        self._nb = [(Buf("sq0"), Buf("tmp0")), (Buf("sq1"), Buf("tmp1"))]
        self.NS = 6
        self.wslot = [A.bf16(4096) for _ in range(self.NS)]
        self.wbuf = [Buf("w%d" % i) for i in range(self.NS)]
        self.wsem = [P.new_sem("wsem%d" % i) for i in range(self.NS)]

        def pf(i4):
            self.plan_ffn(wg[i4 * D:(i4 + 1) * D], wu[i4 * D:(i4 + 1) * D], wd[i4 * DFF:(i4 + 1) * DFF])
        sa = self.stop_after
        pf(0)
        if sa != "ffn00":
            self.plan_mixer0()
        if sa not in ("ffn00", "mix0"):
            pf(1)
        if sa not in ("ffn00", "mix0", "l0"):
            pf(2)
            self.plan_mixer1()
            if sa != "l1a":
                pf(3)

        for t in TILES_H:
            c0, n = t
            P.dma("sp", None, xs[:, :, c0:c0 + n], xT.rearrange("(c p) t -> p c t", p=128)[:, :, c0:c0 + n],
                  writes=(self.xb[t],))
        b_cond = P.buf("cond")
        P.dma("sp", None, cond, cT, writes=(b_cond,))
        P.dma("sp", None, ng, normg, writes=(b_const,))
        P.op("dve", lambda g: g.memset(epsc, EPS), writes=(b_const,))
        P.op("dve", lambda g: g.memset(ones_bf, 1.0), writes=(b_const,))
        self.w_pump()

        A.push()
        awb = [A.f32(6 * 8 * 128).rearrange("p (i k f) -> p i k f", i=6, k=8) for _ in range(2)]
        ab = A.f32(24)[:, 0:18]
        msb = A.f32(24)[:, 0:18]
        b_aw = [P.buf("aw%d" % i) for i in range(2)]
        s_awq = [P.new_sem("awq0"), P.new_sem("awq1")]

        def load_aw(i):
            P.dma("sp", s_awq[i % 2], awb[i % 2], adaw[6 * i:6 * i + 6].rearrange("i (k p) f -> p i k f", p=128),
                  writes=(b_aw[i % 2],))
        load_aw(0)
        load_aw(1)
        b_ab = P.buf("ab")
        P.dma("sp", None, ab, adab, writes=(b_ab,))
        self.act(cond, cond, AF.Silu, reads=(b_cond,), writes=(b_cond,))
        for i in range(18):
            self.mm([(ps[:, 0, i:i + 1], awb[(i // 6) % 2][:, i % 6, k, :], cond[:, k:k + 1], k == 0, k == 7)
                     for k in range(8)], reads=(b_cond, b_aw[(i // 6) % 2]), writes=(self.psb[0],))
            if i == 5:
                load_aw(2)
        b_ms = P.buf("ms")
        P.op("dve", lambda g: g.tensor_tensor(msb, ps[:, 0, 0:18], ab, ALU.add),
             reads=(self.psb[0], b_ab), writes=(b_ms,))
        b_msd = P.buf("msd")
        P.dma("sp", None, mod_send, msb, reads=(b_ms,), writes=(b_msd,))
        b_mall = P.buf("mall")
        s_cc = P.new_sem("cc")
        P.custom("pool", s_cc, 1,
                 lambda g: g.collective_compute("AllGather", ALU.bypass, replica_groups=[list(range(NCORES))],
                                                ins=[mod_send[:, :]], outs=[mod_all[:, :]]),
                 reads=(b_msd,), writes=(b_mall,))
        P.dma("sp", None, modT.rearrange("p (r i) -> p r i", r=NCORES),
              mod_all.rearrange("(r p) i -> p r i", p=128), reads=(b_mall,), writes=(b_mod,))
        A.pop()
        for L in range(2):
            for s in range(3):
                base = L * 72 + s * 24
                o = (L * 3 + s) * 8
                gidx = L * 3 + {0: 0, 1: 1, 2: 2}[s]
                P.op("dve", lambda g, base=base, o=o, gidx=gidx: g.scalar_tensor_tensor(
                    out=vecA[:, o:o + 8], in0=modT[:, base + 8:base + 16], scalar=1.0, in1=ng[:, gidx, :],
                    op0=ALU.add, op1=ALU.mult), reads=(b_mod, b_const), writes=(b_mod,))
                P.op("dve", lambda g, base=base, o=o, s=s: g.tensor_scalar(
                    vecG[:, o:o + 8], modT[:, base + 16:base + 24], 1.0 if s == 1 else 0.5, None, ALU.mult),
                    reads=(b_mod,), writes=(b_mod,))

        self.ffn(0, 0, TILES_H)
        if sa != "ffn00":
            self.mixer0()
        if sa not in ("ffn00", "mix0"):
            self.ffn(0, 2, TILES_H)
        if sa not in ("ffn00", "mix0", "l0"):
            self.ffn(1, 0, TILES_H)
            self.mixer1()
            if sa != "l1a":
                self.ffn(1, 2, TILES)

        s_out = [P.new_sem("out0"), P.new_sem("out1")]
        self.scoped.extend(s_out) if hasattr(self, "scoped") else None
        A.push()
        ost = [A.f32(8 * 512).rearrange("p (c t) -> p c t", c=8) for _ in range(2)]
        ob = [P.buf("ost0"), P.buf("ost1")]
        for ti, t in enumerate(TILES):
            c0, n = t
            self.norm_tile(t, 6, None, None, ost[ti % 2], ob[ti % 2], out_dtype_f32=True)
            P.dma("sp", s_out[ti % 2], outT.rearrange("(c p) t -> p c t", p=128)[:, :, c0 - HALO:c0 - HALO + n],
                  ost[ti % 2][:, :, 0:n], reads=(ob[ti % 2],), writes=())
        A.pop()
        P.emit(nc, [(sm, sm.count) for sm in s_out])
        st.close()
        return nc

    def norm_tile(self, t, gidx, vA, vS, dst, dstbuf, out_dtype_f32=False):
        P, ps, xs, A = self.P, self.ps, self.xs, self.A
        c00, nn = t
        xb = self.xb[t]
        pbank = self.psb[0]
        for pi, o in enumerate(range(0, nn, 256)):
            n = min(256, nn - o)
            c0 = c00 + o
            sq, tmp = self.n_sq[pi % 2], self.n_tmp[0]
            b_sq, b_tmp = self._nb[pi % 2][0], self._nb[0][1]
            pcol = ps[:, 0, (pi % 2) * 256:(pi % 2) * 256 + n]
            self.act(sq[:, :, 0:n], xs[:, :, c0:c0 + n], AF.Square, reads=(xb,), writes=(b_sq,))
            self.mm([(pcol, self.ones_bf, sq[:, c, 0:n], c == 0, c == 7) for c in range(8)],
                    reads=(b_sq, self.b_const), writes=(pbank,))
            self.act(pcol, pcol, AF.Sqrt, reads=(pbank, self.b_const), writes=(pbank,),
                     bias=self.epsc[:, 0:1], scale=1.0 / D)
            P.op("dve", lambda g, pcol=pcol: g.reciprocal(pcol, pcol), reads=(pbank,), writes=(pbank,))
            P.op("dve", lambda g, pcol=pcol, tmp=tmp, c0=c0, n=n: g.tensor_tensor(
                tmp[:, :, 0:n], xs[:, :, c0:c0 + n], pcol.unsqueeze(1).broadcast_to([128, 8, n]), ALU.mult),
                reads=(xb, pbank), writes=(b_tmp,))
            for c in range(8):
                if vA is None:
                    self.act(dst[:, c, o:o + n], tmp[:, c, 0:n], AF.Identity, reads=(b_tmp, self.b_const),
                             writes=(dstbuf,), scale=self.ng[:, gidx, c:c + 1])
                else:
                    self.act(dst[:, c, o:o + n], tmp[:, c, 0:n], AF.Identity, reads=(b_tmp, self.b_mod),
                             writes=(dstbuf,), scale=vA[:, c:c + 1], bias=vS[:, c:c + 1])

    def plan_mixer0(self):
        win, b_win = self.w_cin
        wout, b_wout = self.w_cout
        v5 = win.rearrange("(k p) (s j f) -> p k s j f", p=128, s=5, j=4)
        for q in range(4):
            for j in range(4):
                self.wplan([(v5[:, :, si, j, :], (lambda v, si=si: v[:, :, si, :])) for si in range(2)],
                           lambda sl: sl[:, 0:2048].rearrange("p (k s f) -> p k s f", k=8, s=2), b_win)
                self.wplan([(v5[:, :, 2 + si, j, :], (lambda v, si=si: v[:, :, si, :])) for si in range(3)],
                           lambda sl: sl[:, 0:3072].rearrange("p (k s f) -> p k s f", k=8, s=3), b_win)
            for dg in range(2):
                self.wplan(wout.rearrange("(k p) d -> p k d", p=128)[:, :, dg * 512:(dg + 1) * 512],
                           lambda sl: sl.rearrange("p (k d) -> p k d", k=8), b_wout)

    def mixer0(self):
        P, ps, xs, A = self.P, self.ps, self.xs, self.A
        psb = self.psb
        o = (0 * 3 + 1) * 8
        vA = self.vecA[:, o:o + 8]
        vG = self.vecG[:, o:o + 8]
        vS = self.modT[:, 24:32]
        A.push()
        hT = A.bf16(8 * TT).rearrange("p (c t) -> p c t", c=8)
        hb = {t: P.buf("h%d" % t[0]) for t in TILES_H}
        W, NC = 576, 546
        aj = [(A.f32(W), P.buf("aj")) for _ in range(2)]
        bgj = [(A.f32(W), P.buf("bgj")) for _ in range(2)]
        cbj = [(A.f32(W), P.buf("cbj")) for _ in range(2)]
        acc, acc_b = A.f32(NC), P.buf("acc")
        cbc, cbc_b = A.f32(NC), P.buf("cbc")
        scr = [(A.f32(512), P.buf("scr%d" % i)) for i in range(6)]
        (sgm, sgm_b), (cgs, cgs_b), (sq_, sq_b), (d_, d_b), (v_, v_b), (r_, r_b) = scr
        mix = A.bf16(8 * W).rearrange("p (c t) -> p c t", c=8)
        mixb = [P.buf("mix%d" % i) for i in range(8)]
        Bd = A.f32(128)
        cv = A.f32(4 * 37).rearrange("p (j v) -> p j v", j=4)
        hm = A.f32(64)
        b_c0 = P.buf("m0const")
        P.op("dve", lambda g: g.memset(Bd, 0.0), writes=(b_c0,))
        P.op("dve", lambda g: g.memset(Bd[0:64, 0:64], 1.0 / 64), writes=(b_c0,))
        P.op("dve", lambda g: g.memset(Bd[64:128, 64:128], 1.0 / 64), writes=(b_c0,))
        b_cv = P.buf("cv")
        P.dma("sp", None, cv, self.cvec_d, writes=(b_cv,))
        P.dma("sp", None, hm, self.hmask_d, writes=(b_cv,))
        for t in TILES_H:
            c0, n = t
            self.norm_tile(t, None, vA, vS, hT[:, :, c0:c0 + n], hb[t])

        def htile(c0):
            for t in TILES_H:
                if t[0] <= c0 < t[0] + t[1]:
                    return hb[t]

        ycnt = 0
        for q in range(4):
            base = 512 * q
            tin = [(base, 64), (base + 64, 512)]
            for j in range(4):
                w2, w2b = self.w_get()
                w3, w3b = self.w_get()
                sl = (q * 4 + j) % 2
                (a_, a_b), (bg_, bg_b), (cb_, cb_b) = aj[sl], bgj[sl], cbj[sl]
                for (c0, n) in tin:
                    r0 = c0 - base
                    hbuf = htile(c0)
                    for si, bank in ((0, 1), (1, 2)):
                        self.mm([(ps[:, bank, 0:n], w2[:, k, si, :], hT[:, k, c0:c0 + n], k == 0, k == 7)
                                 for k in range(8)], reads=(w2b, hbuf), writes=(psb[bank],))
                    self.act(sgm[:, 0:n], ps[:, 2, 0:n], AF.Sigmoid, reads=(psb[2],), writes=(sgm_b,))
                    P.op("dve", lambda g, a_=a_, r0=r0, n=n: g.tensor_tensor(
                        a_[:, r0:r0 + n], ps[:, 1, 0:n], sgm[:, 0:n], ALU.mult),
                        reads=(psb[1], sgm_b), writes=(a_b,))
                    for si, bank in ((0, 3), (1, 4), (2, 5)):
                        self.mm([(ps[:, bank, 0:n], w3[:, k, si, :], hT[:, k, c0:c0 + n], k == 0, k == 7)
                                 for k in range(8)], reads=(w3b, hbuf), writes=(psb[bank],))
                    self.act(bg_[:, r0:r0 + n], ps[:, 3, 0:n], AF.Identity, reads=(psb[3],), writes=(bg_b,))
                    self.act(cgs[:, 0:n], ps[:, 4, 0:n], AF.Identity, reads=(psb[4],), writes=(cgs_b,))
                    P.op("dve", lambda g, cb_=cb_, r0=r0, n=n: g.tensor_tensor(
                        cb_[:, r0:r0 + n], cgs[:, 0:n], ps[:, 5, 0:n], ALU.mult),
                        reads=(psb[5], cgs_b), writes=(cb_b,))
                    if c0 == 0:
                        P.op("dve", lambda g, a_=a_: g.tensor_tensor(a_[:, 0:64], a_[:, 0:64], hm, ALU.mult),
                             reads=(b_cv,), writes=(a_b,))
                        P.op("dve", lambda g, cb_=cb_: g.tensor_tensor(cb_[:, 0:64], cb_[:, 0:64], hm, ALU.mult),
                             reads=(b_cv,), writes=(cb_b,))
                self.w_release(2)
                P.op("dve", lambda g, a_=a_, j=j: g.tensor_scalar(
                    acc[:, 0:NC], a_[:, 0:NC], cv[:, j, 0:1], cv[:, j, 31:32], ALU.mult, ALU.add),
                    reads=(a_b, b_cv), writes=(acc_b,))
                for k in range(1, 31):
                    P.op("dve", lambda g, a_=a_, j=j, k=k: g.scalar_tensor_tensor(
                        out=acc[:, 0:NC], in0=a_[:, k:k + NC], scalar=cv[:, j, k:k + 1], in1=acc[:, 0:NC],
                        op0=ALU.mult, op1=ALU.add), reads=(a_b, b_cv), writes=(acc_b,))
                P.op("dve", lambda g, cb_=cb_, j=j: g.tensor_scalar(
                    cbc[:, 0:NC], cb_[:, 28:28 + NC], cv[:, j, 34:35], None, ALU.mult),
                    reads=(cb_b, b_cv), writes=(cbc_b,))
                for k in (1, 2):
                    P.op("dve", lambda g, cb_=cb_, j=j, k=k: g.scalar_tensor_tensor(
                        out=cbc[:, 0:NC], in0=cb_[:, 28 + k:28 + k + NC], scalar=cv[:, j, 34 + k:35 + k],
                        in1=cbc[:, 0:NC], op0=ALU.mult, op1=ALU.add), reads=(cb_b, b_cv), writes=(cbc_b,))
                P.op("dve", lambda g, bg_=bg_, j=j: g.tensor_tensor(
                    mix[:, 4 + j, 0:NC], bg_[:, 30:30 + NC], cbc[:, 0:NC], ALU.mult),
                    reads=(bg_b, cbc_b), writes=(mixb[4 + j],))
                for (q0, qn) in ((0, 512), (512, NC - 512)):
                    self.act(sq_[:, 0:qn], acc[:, q0:q0 + qn], AF.Square, reads=(acc_b,), writes=(sq_b,))
                    self.mm([(ps[:, 6, 0:qn], Bd, acc[:, q0:q0 + qn], True, True)],
                            reads=(acc_b, b_c0), writes=(psb[6],))
                    self.mm([(ps[:, 0, 0:qn], Bd, sq_[:, 0:qn], True, True)],
                            reads=(sq_b, b_c0), writes=(psb[0],))
                    P.op("dve", lambda g, q0=q0, qn=qn: g.tensor_tensor(
                        d_[:, 0:qn], acc[:, q0:q0 + qn], ps[:, 6, 0:qn], ALU.subtract),
                        reads=(acc_b, psb[6]), writes=(d_b,))
                    self.act(v_[:, 0:qn], ps[:, 6, 0:qn], AF.Square, reads=(psb[6],), writes=(v_b,))
                    P.op("dve", lambda g, qn=qn: g.tensor_tensor(
                        v_[:, 0:qn], ps[:, 0, 0:qn], v_[:, 0:qn], ALU.subtract),
                        reads=(psb[0], v_b), writes=(v_b,))
                    self.act(v_[:, 0:qn], v_[:, 0:qn], AF.Sqrt, reads=(v_b, self.b_const), writes=(v_b,),
                             bias=self.epsc[:, 0:1])
                    P.op("dve", lambda g, qn=qn: g.reciprocal(r_[:, 0:qn], v_[:, 0:qn]),
                         reads=(v_b,), writes=(r_b,))
                    P.op("dve", lambda g, qn=qn: g.tensor_tensor(d_[:, 0:qn], d_[:, 0:qn], r_[:, 0:qn], ALU.mult),
                         reads=(r_b,), writes=(d_b,))
                    self.act(mix[:, j, q0:q0 + qn], d_[:, 0:qn], AF.Silu, reads=(d_b, b_cv), writes=(mixb[j],),
                             scale=cv[:, j, 32:33], bias=cv[:, j, 33:34])
            if q == 0:
                upd = [(30, 34, TILES_H[0]), (64, 512, TILES_H[1])]
            else:
                upd = [(base + 64, 512, TILES_H[q + 1])]
            for dg in range(2):
                wo, wob = self.w_get()
                for (ac0, n, xt) in upd:
                    i0 = ac0 - base - 30
                    for dd in range(4):
                        dc = dg * 4 + dd
                        yb = 1 + (ycnt % 2)
                        ycnt += 1
                        self.mm([(ps[:, yb, 0:n], wo[:, kc, dd * 128:(dd + 1) * 128], mix[:, kc, i0:i0 + n],
                                  kc == 0, kc == 7) for kc in range(8)],
                                reads=tuple(mixb) + (wob,), writes=(psb[yb],))
                        P.op("dve", lambda g, yb=yb, dc=dc, ac0=ac0, n=n: g.scalar_tensor_tensor(
                            out=xs[:, dc, ac0:ac0 + n], in0=ps[:, yb, 0:n], scalar=vG[:, dc:dc + 1],
                            in1=xs[:, dc, ac0:ac0 + n], op0=ALU.mult, op1=ALU.add),
                            reads=(psb[yb], self.b_mod), writes=(self.xb[xt],))
                self.w_release(1)
        A.pop()

    def plan_mixer1(self):
        win, b_win = self.w_pin
        wout, b_wout = self.w_pout
        vin = win.rearrange("(k p) f -> p k f", p=128)
        v8 = (lambda sl: sl.rearrange("p (k f) -> p k f", k=8))
        v4 = (lambda sl: sl.rearrange("p (k d) -> p k d", k=4))
        self.wplan(vin[:, :, 0:512], v8, b_win)
        self.wplan(wout.rearrange("(k p) d -> p k d", p=128)[:, 0:4, :], v4, b_wout)
        self.wplan(vin[:, :, 512:1024], v8, b_win)
        self.wplan(vin[:, :, 1024:1536], v8, b_win)
        self.wplan(vin[:, :, 1536:2048], v8, b_win)
        self.wplan(wout.rearrange("(k p) d -> p k d", p=128)[:, 4:8, :], v4, b_wout)

    def mixer1(self):
        nc, P, ps, xs, A = self.nc, self.P, self.ps, self.xs, self.A
        psb = self.psb
        o = (1 * 3 + 1) * 8
        vA = self.vecA[:, o:o + 8]
        vG = self.vecG[:, o:o + 8]
        vS = self.modT[:, 72 + 24:72 + 32]
        sQK = nc.dram_tensor("sQK", [8 * 2 * 64, TOK], BF16, kind="Internal").ap()
        aQK = nc.dram_tensor("aQK", [NCORES * 8 * 2 * 64, TOK], BF16, kind="Internal").ap()
        sV = nc.dram_tensor("sV", [8 * 16 * 128, 64], BF16, kind="Internal").ap()
        aV = nc.dram_tensor("aV", [NCORES * 8 * 16 * 128, 64], BF16, kind="Internal").ap()
        sA_ = nc.dram_tensor("sAtt", [64, SEQ], BF16, kind="Internal").ap()
        aA = nc.dram_tensor("aAtt", [NCORES * 64, SEQ], BF16, kind="Internal").ap()
        b_sQK, b_sV, b_aQK, b_aV, b_sA, b_aA = (Buf("sQK"), Buf("sV"), Buf("aQK"), Buf("aV"), Buf("sA"), Buf("aA"))

        A.push()
        hT = A.bf16(8 * TT).rearrange("p (c t) -> p c t", c=8)
        hb = {t: P.buf("h%d" % t[0]) for t in TILES_H}
        ug, ug_b = A.f32(TT), P.buf("ug")
        HW_ = 1040
        sA, sA_b = A.f32(HW_), P.buf("sA")
        sB, sB_b = A.f32(HW_), P.buf("sB")
        pmx = [(A.bf16(1024), P.buf("pmx%d" % i)) for i in range(2)]
        stg = [(A.bf16(TOK), P.buf("stg%d" % i)) for i in range(2)]
        vst = [(A.bf16(512), P.buf("vst%d" % i)) for i in range(2)]
        pw = A.f32(4 * 128).rearrange("p (g d) -> p g d", g=4)
        pv = A.f32(8).rearrange("p (g v) -> p g v", g=4)
        pc = A.f32(64).rearrange("p (g t) -> p g t", g=4)
        hm = A.f32(64)
        b_pc = P.buf("pconst")
        P.dma("sp", None, pw, self.poolw_d.rearrange("g c d -> c g d"), writes=(b_pc,))
        P.dma("sp", None, pv, self.pvec_d, writes=(b_pc,))
        P.dma("sp", None, pc, self.pcorr_d, writes=(b_pc,))
        P.dma("sp", None, hm, self.hmask_d, writes=(b_pc,))
        for t in TILES_H:
            c0, n = t
            self.norm_tile(t, None, vA, vS, hT[:, :, c0:c0 + n], hb[t])
        wu_, wub = self.w_get()
        wo1, wo1b = self.w_get()
        ycnt = 0
        for g_ in range(4):
            win_ = (2, 4, 8, 16)[g_]
            for bi, t in enumerate(TILES_H):
                c0, n = t
                bank = 1 + bi % 2
                self.mm([(ps[:, bank, 0:n], wu_[:, k, g_ * 128:(g_ + 1) * 128], hT[:, k, c0:c0 + n], k == 0, k == 7)
                         for k in range(8)], reads=(wub, hb[t]), writes=(psb[bank],))
                self.act(ug[:, c0:c0 + n], ps[:, bank, 0:n], AF.Identity, reads=(psb[bank],), writes=(ug_b,))
            P.op("dve", lambda g: g.tensor_tensor(ug[:, 0:64], ug[:, 0:64], hm, ALU.mult),
                 reads=(b_pc,), writes=(ug_b,))
            for hp in range(2):
                b0 = 48 + hp * 1024
                src, src_b = None, None
                bufs = [(sA, sA_b), (sB, sB_b)]
                step = 1
                cur = None
                ki = 0
                while step < win_:
                    dst, dst_b = bufs[ki % 2]
                    if cur is None:
                        P.op("dve", lambda g, dst=dst, b0=b0: g.tensor_tensor(
                            dst[:, 1:HW_], ug[:, b0 + 1:b0 + HW_], ug[:, b0:b0 + HW_ - 1], ALU.add),
                            reads=(ug_b,), writes=(dst_b,))
                    else:
                        c_, c_b = cur
                        P.op("dve", lambda g, dst=dst, c_=c_, step=step: g.tensor_tensor(
                            dst[:, step:HW_], c_[:, step:HW_], c_[:, 0:HW_ - step], ALU.add),
                            reads=(c_b,), writes=(dst_b,))
                    cur = (dst, dst_b)
                    step *= 2
                    ki += 1
                c_, c_b = cur
                dst, dst_b = bufs[ki % 2]
                if hp == 0:
                    P.op("dve", lambda g, c_=c_, g_=g_: g.tensor_tensor(
                        c_[:, 16:32], c_[:, 16:32], pc[:, g_, :], ALU.mult), reads=(b_pc,), writes=(c_b,))
                P.op("dve", lambda g, dst=dst, c_=c_, b0=b0, win_=win_: g.scalar_tensor_tensor(
                    out=dst[:, 16:HW_], in0=c_[:, 16:HW_], scalar=1.0 / win_, in1=ug[:, b0 + 16:b0 + HW_],
                    op0=ALU.mult, op1=ALU.subtract), reads=(c_b, ug_b), writes=(dst_b,))
                pm, pm_b = pmx[(g_ * 2 + hp) % 2]
                for ti in range(2):
                    bank = 3 + ti
                    self.mm([(ps[:, bank, 0:512], pw[:, g_, :], dst[:, 16 + ti * 512:16 + (ti + 1) * 512], True, True)],
                            reads=(dst_b, b_pc), writes=(psb[bank],))
                    P.op("dve", lambda g, pm=pm, ti=ti, bank=bank, g_=g_: g.tensor_scalar(
                        pm[:, ti * 512:(ti + 1) * 512], ps[:, bank, 0:512], pv[:, g_, 0:1], pv[:, g_, 1:2],
                        ALU.add, ALU.mult), reads=(psb[bank], b_pc), writes=(pm_b,))
                for ti in range(2):
                    t = TILES[hp * 2 + ti]
                    c0, n = t
                    for dc in range(8):
                        yb = 5 + (ycnt % 2)
                        ycnt += 1
                        self.mm([(ps[:, yb, 0:n], wo1[:, g_, dc * 128:(dc + 1) * 128], pm[:, ti * 512:(ti + 1) * 512],
                                  True, True)], reads=(pm_b, wo1b), writes=(psb[yb],))
                        P.op("dve", lambda g, yb=yb, dc=dc, c0=c0, n=n: g.scalar_tensor_tensor(
                            out=xs[:, dc, c0:c0 + n], in0=ps[:, yb, 0:n], scalar=vG[:, dc:dc + 1],
                            in1=xs[:, dc, c0:c0 + n], op0=ALU.mult, op1=ALU.add),
                            reads=(psb[yb], self.b_mod), writes=(self.xb[t],))
        self.w_release(2)
        sQKv = sQK.rearrange("(h i d) t -> h i d t", h=8, i=2)
        for i_ in range(2):
            w_, wb_ = self.w_get()
            for hc in range(4):
                sg_, sg_b = stg[(i_ * 4 + hc) % 2]
                for bi, t in enumerate(TILES):
                    c0, n = t
                    bank = 1 + bi % 2
                    self.mm([(ps[:, bank, 0:n], w_[:, k, hc * 128:(hc + 1) * 128], hT[:, k, c0:c0 + n], k == 0, k == 7)
                             for k in range(8)], reads=(wb_, hb[t]), writes=(psb[bank],))
                    self.act(sg_[:, c0 - HALO:c0 - HALO + n], ps[:, bank, 0:n], AF.Identity, reads=(psb[bank],),
                             writes=(sg_b,), scale=(0.125 if i_ == 0 else 1.0))
                for hh in range(2):
                    P.dma("sp", None, sQKv[2 * hc + hh, i_], sg_[hh * 64:(hh + 1) * 64, :], reads=(sg_b,),
                          writes=(b_sQK,))
            self.w_release(1)
        wv_, wvb = self.w_get()
        sVv = sV.rearrange("(h ts p) d -> ts p h d", h=8, ts=16)
        for ts in range(16):
            c0 = HALO + ts * 128
            t = TILES[ts // 4]
            vs_, vs_b = vst[ts % 2]
            bank = 1 + ts % 2
            self.mm([(ps[:, bank, 0:512], hT[:, k, c0:c0 + 128], wv_[:, k, :], k == 0, k == 7) for k in range(8)],
                    reads=(wvb, hb[t]), writes=(psb[bank],))
            self.act(vs_, ps[:, bank, 0:512], AF.Identity, reads=(psb[bank],), writes=(vs_b,))
            P.dma("sp", None, sVv[ts], vs_.rearrange("p (h d) -> p h d", h=8), reads=(vs_b,), writes=(b_sV,))
        self.w_release(1)
        A.pop()
        s_cc = P.new_sem("ccx")
        P.custom("pool", s_cc, 1,
                 lambda g: g.collective_compute("AllGather", ALU.bypass, replica_groups=[list(range(NCORES))],
                                                ins=[sQK[:, :]], outs=[aQK[:, :]]),
                 reads=(b_sQK,), writes=(b_aQK,))
        P.custom("pool", s_cc, 1,
                 lambda g: g.collective_compute("AllGather", ALU.bypass, replica_groups=[list(range(NCORES))],
                                                ins=[sV[:, :]], outs=[aV[:, :]]),
                 reads=(b_sV,), writes=(b_aV,))

        if self.stop_after == "l1a":
            return
        A.push()
        NB = 64
        Kaug = A.bf16(SEQ)
        V_sb = A.bf16(128 * 65).rearrange("p (k d) -> p k d", d=65)
        QA = [(A.bf16(512), P.buf("QA%d" % i), P.buf("QAm%d" % i)) for i in range(2)]
        QB = [(A.bf16(512), P.buf("QB%d" % i), P.buf("QBm%d" % i)) for i in range(2)]
        pt = [(A.bf16(512), P.buf("pt%d" % i)) for i in range(3)]
        km = A.f32(64)
        kmh = A.bf16(64)
        kml = A.bf16(64)
        kab = A.f32(8)
        kabh = A.bf16(8)
        ac = A.f32(4)
        drev = A.f32(127)
        tri = A.bf16(512).rearrange("p (s q) -> p s q", s=2)
        ident = A.bf16(128)
        onesf = A.f32(64)
        gsb = [(A.f32(64), P.buf("gsb%d" % i)) for i in range(2)]
        Mf = [(A.f32(64), P.buf("Mf%d" % i)) for i in range(2)]
        Mh = [(A.bf16(256), P.buf("Mh%d" % i)) for i in range(2)]
        m8 = [(A.f32(8), P.buf("m8%d" % i)) for i in range(2)]
        bq = [(A.f32(8), P.buf("bq%d" % i)) for i in range(2)]
        absq = [(A.bf16(128), P.buf("absq%d" % i)) for i in range(2)]
        osb = [(A.f32(512), P.buf("osb%d" % i)) for i in range(2)]
        rd = [(A.f32(512), P.buf("rd%d" % i)) for i in range(2)]
        ast = [(A.bf16(512), P.buf("ast%d" % i)) for i in range(2)]
        rdh = [(A.bf16(1024), P.buf("rdh%d" % i)) for i in range(2)]
        b_K, b_V, b_ac, b_km = P.buf("K"), P.buf("V"), P.buf("ac"), P.buf("km")

        rank_cache = {}

        def rank_of(g):
            if "r" not in rank_cache:
                rank_cache["r"] = g.partition_id() % NCORES
            return rank_cache["r"]

        aQKv = aQK.rearrange("(r x d) t -> d r x t", r=NCORES, x=16)
        myQ = nc.dram_tensor("myQ", [64, SEQ], BF16, kind="Internal").ap()
        myV = nc.dram_tensor("myV", [SEQ, 64], BF16, kind="Internal").ap()
        b_myQ, b_myV = Buf("myQ"), Buf("myV")
        P.custom("sp", None, 16,
                 lambda g: g.dma_start(out=Kaug[0:64, :].rearrange("p (r t) -> p r t", r=NCORES),
                                       in_=aQKv[:, :, bass.ds(rank_of(g) * 2 + 1, 1), :].rearrange("d r x t -> d (r x) t")),
                 reads=(b_aQK,), writes=(b_K,))
        P.custom("sp", None, 16,
                 lambda g: g.dma_start(out=myQ.rearrange("d (r t) -> d r t", r=NCORES),
                                       in_=aQKv[:, :, bass.ds(rank_of(g) * 2, 1), :].rearrange("d r x t -> d (r x) t")),
                 reads=(b_aQK,), writes=(b_myQ,))
        P.dma("sp", None, Kaug[64:128, :], self.khot_d, writes=(b_K,))
        aVv = aV.rearrange("(r h x) d -> r h x d", r=NCORES, h=8)
        P.custom("sp", None, 16,
                 lambda g: g.dma_start(out=myV.rearrange("(r x) d -> r x d", r=NCORES),
                                       in_=aVv[:, bass.ds(rank_of(g), 1), :, :].rearrange("r h x d -> r (h x) d")),
                 reads=(b_aV,), writes=(b_myV,))
        myVv = myV.rearrange("(r ts p) d -> r p ts d", r=NCORES, ts=16)
        for r in range(NCORES):
            for hf in range(2):
                P.dma("sp", None, V_sb[:, r * 16 + hf * 8:r * 16 + hf * 8 + 8, 0:64], myVv[r][:, hf * 8:hf * 8 + 8, :],
                      reads=(b_myV,), writes=(b_V,))
        P.op("pool", lambda g: g.memset(V_sb[:, :, 64:65], 1.0), writes=(b_V,))
        P.dma("sp", None, ac, self.aconst_d, writes=(b_ac,))
        P.dma("sp", None, drev, self.drev_d, writes=(b_ac,))
        P.dma("sp", None, tri.rearrange("p s q -> p (s q)"), self.tri_d, writes=(b_ac,))
        P.dma("sp", None, ident, self.ident_d, writes=(b_ac,))
        P.op("pool", lambda g: g.memset(onesf, 1.0), writes=(b_ac,))
        for i in range(2):
            P.op("pool", lambda g, i=i: g.memset(Mh[i][0], 0.0), writes=(Mh[i][1],))
        P.op("dve", lambda g: g.tensor_reduce(km[0:64, :], Kaug[0:64, :].rearrange("p (n k) -> p n k", k=256), AX.X, ALU.add),
             reads=(b_K,), writes=(b_km,))
        P.op("dve", lambda g: g.tensor_scalar(km[0:64, :], km[0:64, :], 1.0 / 256, None, ALU.mult),
             reads=(), writes=(b_km,))
        P.op("dve", lambda g: g.tensor_copy(kmh[0:64, :], km[0:64, :]), reads=(), writes=(b_km,))
        P.op("dve", lambda g: g.tensor_tensor(kml[0:64, :], km[0:64, :], kmh[0:64, :], ALU.subtract),
             reads=(), writes=(b_km,))
        P.op("dve", lambda g: g.tensor_reduce(kab[0:64, 0:1], Kaug[0:64, :], AX.X, ALU.max, apply_absolute_value=True),
             reads=(b_K,), writes=(b_km,))
        P.op("dve", lambda g: g.tensor_copy(kabh[0:64, 0:1], kab[0:64, 0:1]), reads=(), writes=(b_km,))

        psT = self.psT

        def gating(qc):
            sl = qc % 2
            qa, qa_b, qam_b = QA[sl]
            qb, qb_b, qbm_b = QB[sl]
            r, lc = qc // 4, (qc % 4) * 512
            useB = (2 * qc + 1) >= 32
            P.dma("sp", None, qa[0:64, :], myQ[:, qc * 512:(qc + 1) * 512], reads=(b_myQ,), writes=(qa_b,))
            if useB:
                P.op("pool", lambda g: g.tensor_copy(qb[0:64, :], qa[0:64, :]), reads=(qa_b,), writes=(qb_b,))
            for tq in range(4):
                qt = qc * 4 + tq
                own = qt // 2
                par = qt % 2
                s2 = qt % 2
                g_, g_b = gsb[s2]
                mf, mf_b = Mf[s2]
                mh, mh_b = Mh[s2]
                m8_, m8_b = m8[s2]
                bq_, bq_b = bq[s2]
                aq_, aq_b = absq[s2]
                qcols = slice(tq * 128, (tq + 1) * 128)
                self.mm([(ps[:, 0, 0:NB], qa[0:64, qcols], kmh[0:64, :], True, False),
                         (ps[:, 0, 0:NB], qa[0:64, qcols], kml[0:64, :], False, True)],
                        reads=(qa_b, b_km), writes=(psb[0],))
                self.act(aq_[0:64, :], qa[0:64, qcols], AF.Abs, reads=(qa_b,), writes=(aq_b,))
                self.mm([(ps[:, 0, 64:65], aq_[0:64, :], kabh[0:64, 0:1], True, True)],
                        reads=(aq_b, b_km), writes=(psb[0],))
                P.op("pool", lambda g, g_=g_: g.memset(g_, NEG), writes=(g_b,))
                P.op("pool", lambda g, mf=mf: g.memset(mf, 0.0), writes=(mf_b,))
                if own > 0:
                    P.op("dve", lambda g, g_=g_, own=own: g.tensor_copy(g_[:, 0:own], ps[:, 0, 0:own]),
                         reads=(psb[0],), writes=(g_b,))
                    P.op("dve", lambda g, g_=g_, m8_=m8_, own=own: g.max(m8_, g_[:, 0:max(own, 8)]),
                         reads=(g_b,), writes=(m8_b,))
                    P.op("dve", lambda g, g_=g_, mf=mf, m8_=m8_, own=own: g.tensor_scalar(
                        mf[:, 0:own], g_[:, 0:own], m8_[:, 2:3], NEG, ALU.is_lt, ALU.mult),
                        reads=(g_b, m8_b), writes=(mf_b,))
                P.op("dve", lambda g, bq_=bq_, par=par: g.tensor_tensor(
                    bq_[:, 0:1], ac[:, par:par + 1], ps[:, 0, 64:65], ALU.subtract),
                    reads=(psb[0], b_ac), writes=(bq_b,))
                P.op("dve", lambda g, mf=mf, bq_=bq_, own=own: g.scalar_tensor_tensor(
                    out=mf, in0=mf, scalar=bq_[:, 0:1], in1=drev[:, 63 - own:127 - own], op0=ALU.add, op1=ALU.add),
                    reads=(bq_b, b_ac), writes=(mf_b,))
                for half, (qx, qxm_b) in enumerate(((qa, qam_b), (qb, qbm_b))):
                    if half == 1 and not useB:
                        continue
                    cb = half * 128
                    P.op("dve", lambda g, mh=mh, mf=mf, cb=cb, half=half: g.tensor_copy(
                        mh[:, cb + 64:cb + 96], mf[:, half * 32:half * 32 + 32]), reads=(mf_b,), writes=(mh_b,))
                    P.op("dve", lambda g, mh=mh, mf=mf, cb=cb, half=half: g.tensor_tensor(
                        mh[:, cb + 96:cb + 128], mf[:, half * 32:half * 32 + 32], mh[:, cb + 64:cb + 96],
                        ALU.subtract), reads=(mf_b,), writes=(mh_b,))
                    tcol = ((qt * 2 + half) % 8) * 128
                    P.op("pe", lambda g, mh=mh, cb=cb, tcol=tcol: g.transpose(
                        psT[:, tcol:tcol + 128], mh[:, cb:cb + 128], ident), reads=(mh_b, b_ac), writes=(self.psTb,))
                    self.P.op("act", lambda g, qx=qx, qcols=qcols, tcol=tcol: g.activation(
                        out=qx[64:128, qcols], in_=psT[64:128, tcol:tcol + 128], func=AF.Identity),
                        reads=(self.psTb,), writes=(qxm_b,))

        def attend(qc):
            sl = qc % 2
            qa, qa_b, qam_b = QA[sl]
            qb, qb_b, qbm_b = QB[sl]
            b0 = 2 * qc
            nkb = 4 * qc + 4
            ob = 4 + qc % 2
            for kb in range(nkb):
                n_ = kb // 2
                if n_ < 32:
                    qx, rds = qa, (qa_b, qam_b)
                else:
                    qx, rds = qb, (qb_b, qbm_b)
                sb_ = 1 + kb % 3
                mms = [(ps[:, sb_, 0:512], Kaug[:, kb * 128:(kb + 1) * 128], qx[:, :], True, not (n_ >= b0))]
                if n_ == b0:
                    mms.append((ps[:, sb_, 0:256], ident, tri[:, kb % 2, :], False, True))
                elif n_ == b0 + 1:
                    mms.append((ps[:, sb_, 256:512], ident, tri[:, kb % 2, :], False, True))
                self.mm(mms, reads=rds + (b_K, b_ac), writes=(psb[sb_],))
                p_, p_b = pt[kb % 3]
                self.act(p_, ps[:, sb_, 0:512], AF.Exp, reads=(psb[sb_], b_ac), writes=(p_b,),
                         bias=ac[:, 2 + kb % 2:3 + kb % 2])
                self.mm([(ps[0:65, ob, 0:512], V_sb[:, kb, :], p_, kb == 0, kb == nkb - 1)],
                        reads=(p_b, b_V), writes=(psb[ob],))

        def finalize(qc):
            ob = 4 + qc % 2
            o_, o_b = osb[qc % 2]
            r_, r_b = rd[qc % 2]
            a_, a_b = ast[qc % 2]
            P.op("dve", lambda g: g.tensor_copy(o_[0:65, :], ps[0:65, ob, 0:512]), reads=(psb[ob],), writes=(o_b,))
            rh_, rh_b = rdh[qc % 2]
            P.op("dve", lambda g: g.reciprocal(r_[64:65, :], o_[64:65, :]), reads=(o_b,), writes=(r_b,))
            P.op("dve", lambda g: g.tensor_copy(rh_[64:65, 0:512], r_[64:65, :]), reads=(r_b,), writes=(rh_b,))
            P.op("dve", lambda g: g.tensor_tensor(rh_[64:65, 512:1024], r_[64:65, :], rh_[64:65, 0:512], ALU.subtract),
                 reads=(r_b,), writes=(rh_b,))
            self.mm([(ps[0:64, 6, 0:512], self.ones_bf[64:65, 0:64], rh_[64:65, 0:512], True, False),
                     (ps[0:64, 6, 0:512], self.ones_bf[64:65, 0:64], rh_[64:65, 512:1024], False, True)],
                    reads=(rh_b, self.b_const), writes=(psb[6],))
            P.op("dve", lambda g: g.tensor_tensor(a_[0:64, :], o_[0:64, :], ps[0:64, 6, 0:512], ALU.mult),
                 reads=(o_b, psb[6]), writes=(a_b,))
            P.dma("sp", None, sA_[:, qc * 512:(qc + 1) * 512], a_[0:64, :], reads=(a_b,), writes=(b_sA,))

        NQC = SEQ // 512
        gating(0)
        for qc in range(NQC):
            attend(qc)
            if qc + 1 < NQC:
                gating(qc + 1)
            finalize(qc)
        A.pop()
        P.custom("pool", s_cc, 1,
                 lambda g: g.collective_compute("AllGather", ALU.bypass, replica_groups=[list(range(NCORES))],
                                                ins=[sA_[:, :]], outs=[aA[:, :]]),
                 reads=(b_sA,), writes=(b_aA,))

        A.push()
        mix2 = A.bf16(4 * TOK).rearrange("p (c t) -> p c t", c=4)
        b_m2 = P.buf("mix2")
        aAv = aA.rearrange("(c p) t -> p c t", p=128)
        P.custom("sp", None, 16,
                 lambda g: g.dma_start(out=mix2, in_=aAv[:, :, bass.ds(rank_of(g) * TOK, TOK)]),
                 reads=(b_aA,), writes=(b_m2,))
        wo2, wo2b = self.w_get()
        ycnt = 0
        for t in TILES:
            c0, n = t
            for dc in range(8):
                yb = 5 + (ycnt % 2)
                ycnt += 1
                self.mm([(ps[:, yb, 0:n], wo2[:, kc, dc * 128:(dc + 1) * 128], mix2[:, kc, c0 - HALO:c0 - HALO + n],
                          kc == 0, kc == 3) for kc in range(4)], reads=(b_m2, wo2b), writes=(psb[yb],))
                P.op("dve", lambda g, yb=yb, dc=dc, c0=c0, n=n: g.scalar_tensor_tensor(
                    out=xs[:, dc, c0:c0 + n], in0=ps[:, yb, 0:n], scalar=vG[:, dc:dc + 1],
                    in1=xs[:, dc, c0:c0 + n], op0=ALU.mult, op1=ALU.add),
                    reads=(psb[yb], self.b_mod), writes=(self.xb[t],))
        self.w_release(1)
        A.pop()

    def plan_ffn(self, wg, wu, wd):
        for f0 in range(0, NFC, 4):
            nf = min(4, NFC - f0)
            vgu = (lambda sl, nf=nf: sl[:, 0:8 * nf * 128].rearrange("p (k f) -> p k f", k=8))
            vd = (lambda sl, nf=nf: sl[:, 0:nf * 1024].rearrange("p (f d) -> p f d", f=nf))
            self.wplan(wg.rearrange("(k p) f -> p k f", p=128)[:, :, f0 * 128:(f0 + nf) * 128], vgu, self.wdeps[0])
            self.wplan(wu.rearrange("(k p) f -> p k f", p=128)[:, :, f0 * 128:(f0 + nf) * 128], vgu, self.wdeps[1])
            self.wplan(wd.rearrange("(f p) d -> p f d", p=128)[:, f0:f0 + nf, :], vd, self.wdeps[2])

    def ffn(self, L, s, tiles):
        P, ps, xs, A = self.P, self.ps, self.xs, self.A
        o = (L * 3 + s) * 8
        vA = self.vecA[:, o:o + 8]
        vG = self.vecG[:, o:o + 8]
        vS = self.modT[:, L * 72 + s * 24:L * 72 + s * 24 + 8]
        A.push()
        hT = A.bf16(8 * TT).rearrange("p (c t) -> p c t", c=8)
        hb = {t: P.buf("h%d" % t[0]) for t in tiles}
        asl = [A.bf16(4 * 512).rearrange("p (f t) -> p f t", f=4) for _ in range(2)]
        ab_ = [P.buf("a0"), P.buf("a1")]
        sg = [A.f32(512) for _ in range(2)]
        sgb = [P.buf("sg0"), P.buf("sg1")]
        for t in tiles:
            c0, n = t
            self.norm_tile(t, None, vA, vS, hT[:, :, c0:c0 + n], hb[t])
        it = 0
        yi = 0
        for f0 in range(0, NFC, 4):
            nf = min(4, NFC - f0)
            wgv, wgb = self.w_get()
            wuv, wub = self.w_get()
            wdv, wdb = self.w_get()
            for t in tiles:
                c0, n = t
                sl = it % 2
                it += 1
                a_t, a_b = asl[sl], ab_[sl]
                for fi in range(nf):
                    gi = 1 + (fi % 2)
                    ui = 3 + (fi % 2)
                    self.mm([(ps[:, gi, 0:n], wgv[:, k, fi * 128:(fi + 1) * 128], hT[:, k, c0:c0 + n], k == 0, k == 7)
                             for k in range(8)], reads=(wgb, hb[t]), writes=(self.psb[gi],))
                    self.mm([(ps[:, ui, 0:n], wuv[:, k, fi * 128:(fi + 1) * 128], hT[:, k, c0:c0 + n], k == 0, k == 7)
                             for k in range(8)], reads=(wub, hb[t]), writes=(self.psb[ui],))
                    sgi = fi % 2
                    self.act(sg[sgi][:, 0:n], ps[:, gi, 0:n], AF.Silu, reads=(self.psb[gi],), writes=(sgb[sgi],))
                    P.op("dve", lambda g, sgi=sgi, ui=ui, fi=fi, a_t=a_t, n=n: g.tensor_tensor(
                        a_t[:, fi, 0:n], sg[sgi][:, 0:n], ps[:, ui, 0:n], ALU.mult),
                        reads=(sgb[sgi], self.psb[ui]), writes=(a_b,))
                for dc in range(8):
                    yb = 5 + (yi % 2)
                    yi += 1
                    self.mm([(ps[:, yb, 0:n], wdv[:, fi, dc * 128:(dc + 1) * 128], a_t[:, fi, 0:n], fi == 0, fi == nf - 1)
                             for fi in range(nf)], reads=(wdb, a_b), writes=(self.psb[yb],))
                    P.op("dve", lambda g, yb=yb, dc=dc, c0=c0, n=n: g.scalar_tensor_tensor(
                        out=xs[:, dc, c0:c0 + n], in0=ps[:, yb, 0:n], scalar=vG[:, dc:dc + 1],
                        in1=xs[:, dc, c0:c0 + n], op0=ALU.mult, op1=ALU.add),
                        reads=(self.psb[yb], self.b_mod), writes=(self.xb[t],))
            self.w_release(3)
        A.pop()


_CACHE = {}
GATHER = False


def _prep_inputs(inp, stop_after=None):
    x = np.asarray(inp["x"], dtype=np.float32)[0]
    c = np.asarray(inp["c"], dtype=np.float32)[0]
    ada_w = np.asarray(inp["ada_w"], dtype=np.float32)
    ada_b = np.asarray(inp["ada_b"], dtype=np.float32)
    ffn_norm = np.asarray(inp["ffn_norm"], dtype=np.float32)
    mix_norm = np.asarray(inp["mix_norm"], dtype=np.float32)
    final_norm = np.asarray(inp["final_norm"], dtype=np.float32)
    xT = np.ascontiguousarray(x.T)
    xTp = np.concatenate([np.zeros((D, HALO), np.float32), xT], axis=1)
    adaw_chunks = ada_w.reshape(2, D, 72, 128).transpose(0, 2, 1, 3).reshape(144, D, 128)
    adab_chunks = ada_b.reshape(144, 128)
    gains = np.stack([ffn_norm[0, 0], mix_norm[0], ffn_norm[0, 1], ffn_norm[1, 0], mix_norm[1],
                      ffn_norm[1, 1], final_norm])
    normg = np.ascontiguousarray(gains.reshape(7, 8, 128).transpose(2, 0, 1))
    shared = {
        "cT": np.ascontiguousarray(c.reshape(8, 128).T),
        "normg": normg,
    }
    caw = np.asarray(inp["conv_a_w"], dtype=np.float32)[0]
    cvec = np.concatenate([
        caw.reshape(31, 4, 128).transpose(2, 1, 0),
        np.asarray(inp["conv_a_b"], dtype=np.float32)[0].reshape(4, 128).T[:, :, None],
        np.asarray(inp["conv_a_ln_g"], dtype=np.float32)[0].reshape(4, 128).T[:, :, None],
        np.asarray(inp["conv_a_ln_b"], dtype=np.float32)[0].reshape(4, 128).T[:, :, None],
        np.asarray(inp["conv_b_w"], dtype=np.float32)[0].reshape(3, 4, 128).transpose(2, 1, 0),
    ], axis=2)
    shared["cvec"] = np.ascontiguousarray(cvec.reshape(128, 4 * 37))
    import ml_dtypes
    bf = ml_dtypes.bfloat16
    shared["pool_w"] = np.ascontiguousarray(np.asarray(inp["pool_w"], dtype=np.float32)[0])
    pb = np.asarray(inp["pool_b"], dtype=np.float32)[0]
    psc = np.asarray(inp["pool_scale"], dtype=np.float32)[0].reshape(4, 128)
    shared["pvec"] = np.ascontiguousarray(np.stack([pb.T, psc.T], axis=2).reshape(128, 8))
    blk = np.arange(SEQ) // 256
    khot = np.zeros((64, SEQ), np.float32)
    khot[blk % 32, np.arange(SEQ)] = 1.0
    khot[32 + blk % 32, np.arange(SEQ)] = 1.0
    shared["khot"] = khot.astype(bf)
    kk = np.arange(128)[:, None, None] + 128 * np.arange(2)[None, :, None]
    qq = np.arange(256)[None, None, :]
    shared["tri"] = np.where(kk > qq, NEG, 0.0).astype(np.float32).reshape(128, 512).astype(bf)
    shared["ident"] = np.eye(128, dtype=np.float32).astype(bf)
    weights = {
        "pm_w_in": np.asarray(inp["pm_w_in"], dtype=np.float32)[0],
        "pm_w_out": np.asarray(inp["pm_w_out"], dtype=np.float32)[0],
        "conv_w_in": np.asarray(inp["conv_w_in"], dtype=np.float32)[0],
        "conv_w_out": np.asarray(inp["conv_w_out"], dtype=np.float32)[0],
        "ffn_wg": np.asarray(inp["ffn_w_gate"], dtype=np.float32).reshape(4 * D, DFF),
        "ffn_wu": np.asarray(inp["ffn_w_up"], dtype=np.float32).reshape(4 * D, DFF),
        "ffn_wd": np.asarray(inp["ffn_w_down"], dtype=np.float32).reshape(4 * DFF, D),
    }
    maps = []
    for r in range(NCORES):
        m = dict(shared)
        m["xT"] = np.ascontiguousarray(xTp[:, r * TOK:r * TOK + TT])
        m["adaw"] = np.ascontiguousarray(adaw_chunks[r * 18:(r + 1) * 18])
        m["adab"] = np.ascontiguousarray(adab_chunks[r * 18:(r + 1) * 18].T)
        m["hmask"] = np.full((128, 64), 0.0 if r == 0 else 1.0, np.float32)
        pcorr = np.ones((4, 16), np.float32)
        if r == 0:
            for gi, w_ in enumerate((2, 4, 8, 16)):
                pcorr[gi] = w_ / np.minimum(np.arange(16) + 1, w_)
        m["pcorr"] = np.ascontiguousarray(np.broadcast_to(pcorr.reshape(1, 64), (128, 64)))
        slope = 2.0 ** (-(r + 1))
        pp = np.arange(128, dtype=np.float64)
        m["aconst"] = np.stack([-slope * pp, -slope * (128 + pp), slope * pp, slope * (128 + pp)], axis=1).astype(np.float32)
        ii = np.arange(127)
        dr = np.where(ii <= 63, -slope * 256.0 * (63 - ii), NEG).astype(np.float32)
        m["drev"] = np.ascontiguousarray(np.broadcast_to(dr[None, :], (128, 127)))
        for k, w in weights.items():
            if GATHER:
                n = w.shape[0] // NCORES
                m[k] = np.ascontiguousarray(w[r * n:(r + 1) * n])
            else:
                m[k] = w
        maps.append(m)
    return maps


def kernel(stop_after=None, **inp):
    key = (stop_after, GATHER)
    if key not in _CACHE:
        _CACHE[key] = Builder(stop_after, GATHER).build()
    nc = _CACHE[key]
    maps = _prep_inputs(inp, stop_after)
    res = run_bass_kernel_spmd(nc, maps, core_ids=list(range(NCORES)))
    outT = np.concatenate([np.asarray(r["outT"]) for r in res.results], axis=1)
    return np.ascontiguousarray(outT.T)[None].astype(np.float32)
```

```python
import numpy as np
from contextlib import ExitStack

import concourse.bass as bass
import concourse.mybir as mybir
from concourse.bass_utils import run_bass_kernel_spmd

F32 = mybir.dt.float32
BF16 = mybir.dt.bfloat16
AF = mybir.ActivationFunctionType
ALU = mybir.AluOpType
AX = mybir.AxisListType

NCORES = 8
D = 1024
SEQ = 16384
TOK = SEQ // NCORES
HALO = 64
TT = TOK + HALO
DFF = 2816
NFC = DFF // 128
EPS = 1.0000001e-6
NEG = -30000.0

TILES_H = [(0, 64), (64, 512), (576, 512), (1088, 512), (1600, 512)]
TILES = TILES_H[1:]


class Sem:
    def __init__(self, name):
        self.name = name
        self.h = None
        self.count = 0


class Buf:
    __slots__ = ("name", "w", "r", "dsem")

    def __init__(self, name=""):
        self.name = name
        self.dsem = None
        self.w = None
        self.r = {}


class Eng:
    def __init__(self, name):
        self.name = name
        self.sem = Sem("e_" + name)
        self.ops = []
        self.known = {}


class Prog:
    def __init__(self):
        self.E = {n: Eng(n) for n in ("pe", "act", "dve", "pool", "sp")}
        self.sems = [e.sem for e in self.E.values()]
        self.frontier = {}
        self.scoped = []

    def buf(self, name=""):
        b = Buf(name)
        b.r = dict(self.frontier)
        return b

    def advance_frontier(self):
        for e in self.E.values():
            if e.sem.count:
                self.frontier[e.sem] = e.sem.count
        for sm in self.scoped:
            if sm.count:
                self.frontier[sm] = sm.count

    def new_sem(self, name):
        s = Sem(name)
        self.sems.append(s)
        return s

    def _wait(self, e, s, v):
        if e.known.get(s, 0) >= v:
            return
        if s is e.sem and v <= e.sem.count - 2:
            return
        e.known[s] = v
        e.ops.append(("wait", s, v))

    def _deps(self, e, reads, writes):
        for b in reads:
            if b.w is not None:
                self._wait(e, *b.w)
        for b in writes:
            if b.w is not None:
                self._wait(e, *b.w)
            for s, v in b.r.items():
                self._wait(e, s, v)

    def _stamp(self, stamp, reads, writes):
        s, v = stamp
        for b in reads:
            if b.r.get(s, 0) < v:
                b.r[s] = v
        for b in writes:
            b.w = stamp
            b.r = {}

    def op(self, eng, fn, reads=(), writes=()):
        e = self.E[eng]
        if eng == "pe":
            known_self = e.known.get(e.sem, 0)
            e.known[e.sem] = 1 << 60
            self._deps(e, reads, writes)
            e.known[e.sem] = known_self
        else:
            self._deps(e, reads, writes)
        e.sem.count += 1
        e.ops.append(("op", fn, e.sem, 1))
        self._stamp((e.sem, e.sem.count), reads, writes)

    def dma(self, q, sem, out, in_, reads=(), writes=()):
        e = self.E[q]
        if sem is None:
            sem = self.bufsem(writes[0])
        self._deps(e, reads, writes)
        sem.count += 16
        e.ops.append(("op", (lambda g, o=out, i=in_: g.dma_start(out=o, in_=i)), sem, 16))
        self._stamp((sem, sem.count), reads, writes)

    def bufsem(self, b):
        if b.dsem is None:
            b.dsem = self.new_sem("d%d" % len(self.sems))
            self.scoped.append(b.dsem)
        return b.dsem

    def custom(self, q, sem, inc, fn, reads=(), writes=()):
        e = self.E[q]
        if sem is None:
            sem = self.bufsem(writes[0])
        self._deps(e, reads, writes)
        sem.count += inc
        e.ops.append(("op", fn, sem, inc))
        self._stamp((sem, sem.count), reads, writes)

    def emit(self, nc, final_waits):
        with ExitStack() as st:
            for s in self.sems:
                s.h = st.enter_context(nc.semaphore(s.name))
            block = st.enter_context(nc.Block())
            handles = {"pe": block.tensor, "act": block.scalar, "dve": block.vector,
                       "pool": block.gpsimd, "sp": block.sync}
            for name, e in self.E.items():
                ops = list(e.ops)
                if name == "sp":
                    for s, v in final_waits:
                        ops.append(("wait", s, v))

                def body(g, ops=ops):
                    for o in ops:
                        if o[0] == "wait":
                            g.wait_ge(o[1].h, o[2])
                        else:
                            ins = o[1](g)
                            ins.then_inc(o[2].h, o[3])
                handles[name](body)


class Arena:
    def __init__(self, ap, nwords, prog=None):
        self.prog = prog
        self.ap = ap
        self.n = nwords
        self.top = 0
        self.marks = []

    def push(self):
        self.marks.append(self.top)

    def pop(self):
        self.top = self.marks.pop()
        if self.prog is not None:
            self.prog.advance_frontier()

    def f32(self, n):
        a = self.ap[:, self.top:self.top + n]
        self.top += (n + 7) // 8 * 8
        assert self.top <= self.n, ("SBUF arena overflow", self.top, self.n)
        return a

    def bf16(self, n):
        w = (n + 1) // 2
        return self.f32(w).bitcast(BF16)[:, 0:n]


class Builder:
    def __init__(self, stop_after=None, gather=False):
        self.stop_after = stop_after
        self.gather = gather
        self.b_nodep = Buf("nodep")
        self.nc = bass.Bass("TRN2", target_bir_lowering=False)
        self.P = Prog()
        self.ins = {}
        self.wtasks = []
        self.wnext = 0
        self.wissued = 0
        self.wreleased = 0

    def din(self, name, shape, dt=F32):
        t = self.nc.dram_tensor(name, list(shape), dt, kind="ExternalInput").ap()
        self.ins[name] = t
        return t

    def win(self, name, rows, cols):
        nc, P = self.nc, self.P
        if not self.gather:
            return self.din(name, [rows, cols]), self.b_nodep
        part = self.din(name, [rows // NCORES, cols])
        full = nc.dram_tensor(name + "_full", [rows, cols], F32, kind="Internal").ap()
        parti = nc.dram_tensor(name + "_pi", [rows // NCORES, cols], F32, kind="Internal").ap()
        b = Buf(name)
        bp = Buf(name + "p")
        P.dma("sp", None, parti, part, writes=(bp,))
        sem = P.new_sem("g_" + name)
        P.custom("pool", sem, 1,
                 lambda g: g.collective_compute("AllGather", ALU.bypass, replica_groups=[list(range(NCORES))],
                                                ins=[parti[:, :]], outs=[full[:, :]]),
                 reads=(bp,), writes=(b,))
        return full, b

    def act(self, out, in_, func, reads, writes, bias=None, scale=None):
        kw = {}
        if bias is not None:
            kw["bias"] = bias
        if scale is not None:
            kw["scale"] = scale
        self.P.op("act", lambda g: g.activation(out=out, in_=in_, func=func, **kw), reads, writes)

    def mm(self, mms, reads, writes):
        def fn(g, mms=mms):
            ins = None
            for (o, l, r, s0, s1) in mms:
                ins = g.matmul(o, l, r, start=s0, stop=s1)
            return ins
        self.P.op("pe", fn, reads, writes)

    def wplan(self, src_ap, view_fn, dep):
        self.wtasks.append((src_ap, view_fn, dep))

    def w_pump(self):
        while self.wissued < len(self.wtasks) and self.wissued < self.wreleased + self.NS:
            i = self.wissued
            src, vf, dep = self.wtasks[i]
            slot = i % self.NS
            if isinstance(src, list):
                for (s_ap, sub) in src:
                    self.P.dma("pool", self.wsem[slot], sub(vf(self.wslot[slot])), s_ap, reads=(dep,),
                               writes=(self.wbuf[slot],))
            else:
                self.P.dma("pool", self.wsem[slot], vf(self.wslot[slot]), src, reads=(dep,),
                           writes=(self.wbuf[slot],))
            self.wissued += 1

    def w_get(self):
        i = self.wnext
        assert i < self.wissued, "weight task not issued (ring too small for this group)"
        self.wnext += 1
        src, vf, dep = self.wtasks[i]
        slot = i % self.NS
        return vf(self.wslot[slot]), self.wbuf[slot]

    def w_release(self, k):
        self.wreleased += k
        self.w_pump()

    def build(self):
        nc, P = self.nc, self.P
        st = ExitStack()
        self.st = st
        xT = self.din("xT", [D, TT])
        cT = self.din("cT", [128, 8])
        adaw = self.din("adaw", [18, D, 128])
        adab = self.din("adab", [128, 18])
        normg = self.din("normg", [128, 7, 8])
        wg, b_wg = self.win("ffn_wg", 4 * D, DFF)
        wu, b_wu = self.win("ffn_wu", 4 * D, DFF)
        wd, b_wd = self.win("ffn_wd", 4 * DFF, D)
        self.wdeps = (b_wg, b_wu, b_wd)
        self.w_cin = self.win("conv_w_in", D, 2560)
        self.w_cout = self.win("conv_w_out", D, D)
        self.cvec_d = self.din("cvec", [128, 4 * 37])
        self.w_pin = self.win("pm_w_in", D, 2048)
        self.w_pout = self.win("pm_w_out", D, D)
        self.poolw_d = self.din("pool_w", [4, 128, 128])
        self.pvec_d = self.din("pvec", [128, 8])
        self.pcorr_d = self.din("pcorr", [128, 64])
        self.khot_d = self.din("khot", [64, SEQ], BF16)
        self.aconst_d = self.din("aconst", [128, 4])
        self.drev_d = self.din("drev", [128, 127])
        self.tri_d = self.din("tri", [128, 512], BF16)
        self.ident_d = self.din("ident", [128, 128], BF16)
        self.hmask_d = self.din("hmask", [128, 64])
        outT = nc.dram_tensor("outT", [D, TOK], F32, kind="ExternalOutput").ap()
        mod_send = nc.dram_tensor("mod_send", [128, 18], F32, kind="Internal").ap()
        mod_all = nc.dram_tensor("mod_all", [NCORES * 128, 18], F32, kind="Internal").ap()

        NW = 212000 // 4
        arena_t = st.enter_context(nc.sbuf_tensor("arena", [128, NW], F32))
        A = Arena(arena_t, NW, P)
        self.A = A
        ps = st.enter_context(nc.psum_tensor("ps", [128, 7, 512], F32))
        self.psT = st.enter_context(nc.psum_tensor("psT", [128, 1024], BF16))
        self.psTb = Buf("psT")
        self.ps = ps
        self.psb = [Buf("ps%d" % i) for i in range(8)]

        xs = A.f32(8 * TT).rearrange("p (c t) -> p c t", c=8)
        self.xs = xs
        self.xb = {t: Buf("x%d" % t[0]) for t in TILES_H}
        modT = A.f32(144)
        vecA = A.f32(48)
        vecG = A.f32(48)
        ng = A.f32(56).rearrange("p (a c) -> p a c", a=7)
        cond = A.f32(8)
        epsc = A.f32(8)
        ones_bf = A.bf16(128)
        self.modT, self.vecA, self.vecG, self.ng = modT, vecA, vecG, ng
        self.epsc, self.ones_bf = epsc, ones_bf
        b_const = Buf("const")
        self.b_const = b_const
        b_mod = Buf("mod")
        self.b_mod = b_mod
        self.n_sq = [A.bf16(8 * 256).rearrange("p (c t) -> p c t", c=8) for _ in range(2)]
        self.n_tmp = [A.f32(8 * 256).rearrange("p (c t) -> p c t", c=8) for _ in range(1)]
        self._nb = [(Buf("sq0"), Buf("tmp0")), (Buf("sq1"), Buf("tmp1"))]
        self.NS = 6
        self.wslot = [A.bf16(4096) for _ in range(self.NS)]
        self.wbuf = [Buf("w%d" % i) for i in range(self.NS)]
        self.wsem = [P.new_sem("wsem%d" % i) for i in range(self.NS)]

        def pf(i4):
            self.plan_ffn(wg[i4 * D:(i4 + 1) * D], wu[i4 * D:(i4 + 1) * D], wd[i4 * DFF:(i4 + 1) * DFF])
        sa = self.stop_after
        pf(0)
        if sa != "ffn00":
            self.plan_mixer0()
        if sa not in ("ffn00", "mix0"):
            pf(1)
        if sa not in ("ffn00", "mix0", "l0"):
            pf(2)
            self.plan_mixer1()
            if sa != "l1a":
                pf(3)

        for t in TILES_H:
            c0, n = t
            P.dma("sp", None, xs[:, :, c0:c0 + n], xT.rearrange("(c p) t -> p c t", p=128)[:, :, c0:c0 + n],
                  writes=(self.xb[t],))
        b_cond = P.buf("cond")
        P.dma("sp", None, cond, cT, writes=(b_cond,))
        P.dma("sp", None, ng, normg, writes=(b_const,))
        P.op("dve", lambda g: g.memset(epsc, EPS), writes=(b_const,))
        P.op("dve", lambda g: g.memset(ones_bf, 1.0), writes=(b_const,))
        self.w_pump()

        A.push()
        awb = [A.f32(6 * 8 * 128).rearrange("p (i k f) -> p i k f", i=6, k=8) for _ in range(2)]
        ab = A.f32(24)[:, 0:18]
        msb = A.f32(24)[:, 0:18]
        b_aw = [P.buf("aw%d" % i) for i in range(2)]
        s_awq = [P.new_sem("awq0"), P.new_sem("awq1")]

        def load_aw(i):
            P.dma("sp", s_awq[i % 2], awb[i % 2], adaw[6 * i:6 * i + 6].rearrange("i (k p) f -> p i k f", p=128),
                  writes=(b_aw[i % 2],))
        load_aw(0)
        load_aw(1)
        b_ab = P.buf("ab")
        P.dma("sp", None, ab, adab, writes=(b_ab,))
        self.act(cond, cond, AF.Silu, reads=(b_cond,), writes=(b_cond,))
        for i in range(18):
            self.mm([(ps[:, 0, i:i + 1], awb[(i // 6) % 2][:, i % 6, k, :], cond[:, k:k + 1], k == 0, k == 7)
                     for k in range(8)], reads=(b_cond, b_aw[(i // 6) % 2]), writes=(self.psb[0],))
            if i == 5:
                load_aw(2)
        b_ms = P.buf("ms")
        P.op("dve", lambda g: g.tensor_tensor(msb, ps[:, 0, 0:18], ab, ALU.add),
             reads=(self.psb[0], b_ab), writes=(b_ms,))
        b_msd = P.buf("msd")
        P.dma("sp", None, mod_send, msb, reads=(b_ms,), writes=(b_msd,))
        b_mall = P.buf("mall")
        s_cc = P.new_sem("cc")
        P.custom("pool", s_cc, 1,
                 lambda g: g.collective_compute("AllGather", ALU.bypass, replica_groups=[list(range(NCORES))],
                                                ins=[mod_send[:, :]], outs=[mod_all[:, :]]),
                 reads=(b_msd,), writes=(b_mall,))
        P.dma("sp", None, modT.rearrange("p (r i) -> p r i", r=NCORES),
              mod_all.rearrange("(r p) i -> p r i", p=128), reads=(b_mall,), writes=(b_mod,))
        A.pop()
        for L in range(2):
            for s in range(3):
                base = L * 72 + s * 24
                o = (L * 3 + s) * 8
                gidx = L * 3 + {0: 0, 1: 1, 2: 2}[s]
                P.op("dve", lambda g, base=base, o=o, gidx=gidx: g.scalar_tensor_tensor(
                    out=vecA[:, o:o + 8], in0=modT[:, base + 8:base + 16], scalar=1.0, in1=ng[:, gidx, :],
                    op0=ALU.add, op1=ALU.mult), reads=(b_mod, b_const), writes=(b_mod,))
                P.op("dve", lambda g, base=base, o=o, s=s: g.tensor_scalar(
                    vecG[:, o:o + 8], modT[:, base + 16:base + 24], 1.0 if s == 1 else 0.5, None, ALU.mult),
                    reads=(b_mod,), writes=(b_mod,))

        self.ffn(0, 0, TILES_H)
        if sa != "ffn00":
            self.mixer0()
        if sa not in ("ffn00", "mix0"):
            self.ffn(0, 2, TILES_H)
        if sa not in ("ffn00", "mix0", "l0"):
            self.ffn(1, 0, TILES_H)
            self.mixer1()
            if sa != "l1a":
                self.ffn(1, 2, TILES)

        s_out = [P.new_sem("out0"), P.new_sem("out1")]
        self.scoped.extend(s_out) if hasattr(self, "scoped") else None
        A.push()
        ost = [A.f32(8 * 512).rearrange("p (c t) -> p c t", c=8) for _ in range(2)]
        ob = [P.buf("ost0"), P.buf("ost1")]
        for ti, t in enumerate(TILES):
            c0, n = t
            self.norm_tile(t, 6, None, None, ost[ti % 2], ob[ti % 2], out_dtype_f32=True)
            P.dma("sp", s_out[ti % 2], outT.rearrange("(c p) t -> p c t", p=128)[:, :, c0 - HALO:c0 - HALO + n],
                  ost[ti % 2][:, :, 0:n], reads=(ob[ti % 2],), writes=())
        A.pop()
        P.emit(nc, [(sm, sm.count) for sm in s_out])
        st.close()
        return nc

    def norm_tile(self, t, gidx, vA, vS, dst, dstbuf, out_dtype_f32=False):
        P, ps, xs, A = self.P, self.ps, self.xs, self.A
        c00, nn = t
        xb = self.xb[t]
        pbank = self.psb[0]
        for pi, o in enumerate(range(0, nn, 256)):
            n = min(256, nn - o)
            c0 = c00 + o
            sq, tmp = self.n_sq[pi % 2], self.n_tmp[0]
            b_sq, b_tmp = self._nb[pi % 2][0], self._nb[0][1]
            pcol = ps[:, 0, (pi % 2) * 256:(pi % 2) * 256 + n]
            self.act(sq[:, :, 0:n], xs[:, :, c0:c0 + n], AF.Square, reads=(xb,), writes=(b_sq,))
            self.mm([(pcol, self.ones_bf, sq[:, c, 0:n], c == 0, c == 7) for c in range(8)],
                    reads=(b_sq, self.b_const), writes=(pbank,))
            self.act(pcol, pcol, AF.Sqrt, reads=(pbank, self.b_const), writes=(pbank,),
                     bias=self.epsc[:, 0:1], scale=1.0 / D)
            P.op("dve", lambda g, pcol=pcol: g.reciprocal(pcol, pcol), reads=(pbank,), writes=(pbank,))
            P.op("dve", lambda g, pcol=pcol, tmp=tmp, c0=c0, n=n: g.tensor_tensor(
                tmp[:, :, 0:n], xs[:, :, c0:c0 + n], pcol.unsqueeze(1).broadcast_to([128, 8, n]), ALU.mult),
                reads=(xb, pbank), writes=(b_tmp,))
            for c in range(8):
                if vA is None:
                    self.act(dst[:, c, o:o + n], tmp[:, c, 0:n], AF.Identity, reads=(b_tmp, self.b_const),
                             writes=(dstbuf,), scale=self.ng[:, gidx, c:c + 1])
                else:
                    self.act(dst[:, c, o:o + n], tmp[:, c, 0:n], AF.Identity, reads=(b_tmp, self.b_mod),
                             writes=(dstbuf,), scale=vA[:, c:c + 1], bias=vS[:, c:c + 1])

    def plan_mixer0(self):
        win, b_win = self.w_cin
        wout, b_wout = self.w_cout
        v5 = win.rearrange("(k p) (s j f) -> p k s j f", p=128, s=5, j=4)
        for q in range(4):
            for j in range(4):
                self.wplan([(v5[:, :, si, j, :], (lambda v, si=si: v[:, :, si, :])) for si in range(2)],
                           lambda sl: sl[:, 0:2048].rearrange("p (k s f) -> p k s f", k=8, s=2), b_win)
                self.wplan([(v5[:, :, 2 + si, j, :], (lambda v, si=si: v[:, :, si, :])) for si in range(3)],
                           lambda sl: sl[:, 0:3072].rearrange("p (k s f) -> p k s f", k=8, s=3), b_win)
            for dg in range(2):
                self.wplan(wout.rearrange("(k p) d -> p k d", p=128)[:, :, dg * 512:(dg + 1) * 512],
                           lambda sl: sl.rearrange("p (k d) -> p k d", k=8), b_wout)

    def mixer0(self):
        P, ps, xs, A = self.P, self.ps, self.xs, self.A
        psb = self.psb
        o = (0 * 3 + 1) * 8
        vA = self.vecA[:, o:o + 8]
        vG = self.vecG[:, o:o + 8]
        vS = self.modT[:, 24:32]
        A.push()
        hT = A.bf16(8 * TT).rearrange("p (c t) -> p c t", c=8)
        hb = {t: P.buf("h%d" % t[0]) for t in TILES_H}
        W, NC = 576, 546
        aj = [(A.f32(W), P.buf("aj")) for _ in range(2)]
        bgj = [(A.f32(W), P.buf("bgj")) for _ in range(2)]
        cbj = [(A.f32(W), P.buf("cbj")) for _ in range(2)]
        acc, acc_b = A.f32(NC), P.buf("acc")
        cbc, cbc_b = A.f32(NC), P.buf("cbc")
        scr = [(A.f32(512), P.buf("scr%d" % i)) for i in range(6)]
        (sgm, sgm_b), (cgs, cgs_b), (sq_, sq_b), (d_, d_b), (v_, v_b), (r_, r_b) = scr
        mix = A.bf16(8 * W).rearrange("p (c t) -> p c t", c=8)
        mixb = [P.buf("mix%d" % i) for i in range(8)]
        Bd = A.f32(128)
        cv = A.f32(4 * 37).rearrange("p (j v) -> p j v", j=4)
        hm = A.f32(64)
        b_c0 = P.buf("m0const")
        P.op("dve", lambda g: g.memset(Bd, 0.0), writes=(b_c0,))
        P.op("dve", lambda g: g.memset(Bd[0:64, 0:64], 1.0 / 64), writes=(b_c0,))
        P.op("dve", lambda g: g.memset(Bd[64:128, 64:128], 1.0 / 64), writes=(b_c0,))
        b_cv = P.buf("cv")
        P.dma("sp", None, cv, self.cvec_d, writes=(b_cv,))
        P.dma("sp", None, hm, self.hmask_d, writes=(b_cv,))
        for t in TILES_H:
            c0, n = t
            self.norm_tile(t, None, vA, vS, hT[:, :, c0:c0 + n], hb[t])

        def htile(c0):
            for t in TILES_H:
                if t[0] <= c0 < t[0] + t[1]:
                    return hb[t]

        ycnt = 0
        for q in range(4):
            base = 512 * q
            tin = [(base, 64), (base + 64, 512)]
            for j in range(4):
                w2, w2b = self.w_get()
                w3, w3b = self.w_get()
                sl = (q * 4 + j) % 2
                (a_, a_b), (bg_, bg_b), (cb_, cb_b) = aj[sl], bgj[sl], cbj[sl]
                for (c0, n) in tin:
                    r0 = c0 - base
                    hbuf = htile(c0)
                    for si, bank in ((0, 1), (1, 2)):
                        self.mm([(ps[:, bank, 0:n], w2[:, k, si, :], hT[:, k, c0:c0 + n], k == 0, k == 7)
                                 for k in range(8)], reads=(w2b, hbuf), writes=(psb[bank],))
                    self.act(sgm[:, 0:n], ps[:, 2, 0:n], AF.Sigmoid, reads=(psb[2],), writes=(sgm_b,))
                    P.op("dve", lambda g, a_=a_, r0=r0, n=n: g.tensor_tensor(
                        a_[:, r0:r0 + n], ps[:, 1, 0:n], sgm[:, 0:n], ALU.mult),
                        reads=(psb[1], sgm_b), writes=(a_b,))
                    for si, bank in ((0, 3), (1, 4), (2, 5)):
                        self.mm([(ps[:, bank, 0:n], w3[:, k, si, :], hT[:, k, c0:c0 + n], k == 0, k == 7)
                                 for k in range(8)], reads=(w3b, hbuf), writes=(psb[bank],))
                    self.act(bg_[:, r0:r0 + n], ps[:, 3, 0:n], AF.Identity, reads=(psb[3],), writes=(bg_b,))
                    self.act(cgs[:, 0:n], ps[:, 4, 0:n], AF.Identity, reads=(psb[4],), writes=(cgs_b,))
                    P.op("dve", lambda g, cb_=cb_, r0=r0, n=n: g.tensor_tensor(
                        cb_[:, r0:r0 + n], cgs[:, 0:n], ps[:, 5, 0:n], ALU.mult),
                        reads=(psb[5], cgs_b), writes=(cb_b,))
                    if c0 == 0:
                        P.op("dve", lambda g, a_=a_: g.tensor_tensor(a_[:, 0:64], a_[:, 0:64], hm, ALU.mult),
                             reads=(b_cv,), writes=(a_b,))
                        P.op("dve", lambda g, cb_=cb_: g.tensor_tensor(cb_[:, 0:64], cb_[:, 0:64], hm, ALU.mult),
                             reads=(b_cv,), writes=(cb_b,))
                self.w_release(2)
                P.op("dve", lambda g, a_=a_, j=j: g.tensor_scalar(
                    acc[:, 0:NC], a_[:, 0:NC], cv[:, j, 0:1], cv[:, j, 31:32], ALU.mult, ALU.add),
                    reads=(a_b, b_cv), writes=(acc_b,))
                for k in range(1, 31):
                    P.op("dve", lambda g, a_=a_, j=j, k=k: g.scalar_tensor_tensor(
                        out=acc[:, 0:NC], in0=a_[:, k:k + NC], scalar=cv[:, j, k:k + 1], in1=acc[:, 0:NC],
                        op0=ALU.mult, op1=ALU.add), reads=(a_b, b_cv), writes=(acc_b,))
                P.op("dve", lambda g, cb_=cb_, j=j: g.tensor_scalar(
                    cbc[:, 0:NC], cb_[:, 28:28 + NC], cv[:, j, 34:35], None, ALU.mult),
                    reads=(cb_b, b_cv), writes=(cbc_b,))
                for k in (1, 2):
                    P.op("dve", lambda g, cb_=cb_, j=j, k=k: g.scalar_tensor_tensor(
                        out=cbc[:, 0:NC], in0=cb_[:, 28 + k:28 + k + NC], scalar=cv[:, j, 34 + k:35 + k],
                        in1=cbc[:, 0:NC], op0=ALU.mult, op1=ALU.add), reads=(cb_b, b_cv), writes=(cbc_b,))
                P.op("dve", lambda g, bg_=bg_, j=j: g.tensor_tensor(
                    mix[:, 4 + j, 0:NC], bg_[:, 30:30 + NC], cbc[:, 0:NC], ALU.mult),
                    reads=(bg_b, cbc_b), writes=(mixb[4 + j],))
                for (q0, qn) in ((0, 512), (512, NC - 512)):
                    self.act(sq_[:, 0:qn], acc[:, q0:q0 + qn], AF.Square, reads=(acc_b,), writes=(sq_b,))
                    self.mm([(ps[:, 6, 0:qn], Bd, acc[:, q0:q0 + qn], True, True)],
                            reads=(acc_b, b_c0), writes=(psb[6],))
                    self.mm([(ps[:, 0, 0:qn], Bd, sq_[:, 0:qn], True, True)],
                            reads=(sq_b, b_c0), writes=(psb[0],))
                    P.op("dve", lambda g, q0=q0, qn=qn: g.tensor_tensor(
                        d_[:, 0:qn], acc[:, q0:q0 + qn], ps[:, 6, 0:qn], ALU.subtract),
                        reads=(acc_b, psb[6]), writes=(d_b,))
                    self.act(v_[:, 0:qn], ps[:, 6, 0:qn], AF.Square, reads=(psb[6],), writes=(v_b,))
                    P.op("dve", lambda g, qn=qn: g.tensor_tensor(
                        v_[:, 0:qn], ps[:, 0, 0:qn], v_[:, 0:qn], ALU.subtract),
                        reads=(psb[0], v_b), writes=(v_b,))
                    self.act(v_[:, 0:qn], v_[:, 0:qn], AF.Sqrt, reads=(v_b, self.b_const), writes=(v_b,),
                             bias=self.epsc[:, 0:1])
                    P.op("dve", lambda g, qn=qn: g.reciprocal(r_[:, 0:qn], v_[:, 0:qn]),
                         reads=(v_b,), writes=(r_b,))
                    P.op("dve", lambda g, qn=qn: g.tensor_tensor(d_[:, 0:qn], d_[:, 0:qn], r_[:, 0:qn], ALU.mult),
                         reads=(r_b,), writes=(d_b,))
                    self.act(mix[:, j, q0:q0 + qn], d_[:, 0:qn], AF.Silu, reads=(d_b, b_cv), writes=(mixb[j],),
                             scale=cv[:, j, 32:33], bias=cv[:, j, 33:34])
            if q == 0:
                upd = [(30, 34, TILES_H[0]), (64, 512, TILES_H[1])]
            else:
                upd = [(base + 64, 512, TILES_H[q + 1])]
            for dg in range(2):
                wo, wob = self.w_get()
                for (ac0, n, xt) in upd:
                    i0 = ac0 - base - 30
                    for dd in range(4):
                        dc = dg * 4 + dd
                        yb = 1 + (ycnt % 2)
                        ycnt += 1
                        self.mm([(ps[:, yb, 0:n], wo[:, kc, dd * 128:(dd + 1) * 128], mix[:, kc, i0:i0 + n],
                                  kc == 0, kc == 7) for kc in range(8)],
                                reads=tuple(mixb) + (wob,), writes=(psb[yb],))
                        P.op("dve", lambda g, yb=yb, dc=dc, ac0=ac0, n=n: g.scalar_tensor_tensor(
                            out=xs[:, dc, ac0:ac0 + n], in0=ps[:, yb, 0:n], scalar=vG[:, dc:dc + 1],
                            in1=xs[:, dc, ac0:ac0 + n], op0=ALU.mult, op1=ALU.add),
                            reads=(psb[yb], self.b_mod), writes=(self.xb[xt],))
                self.w_release(1)
        A.pop()

    def plan_mixer1(self):
        win, b_win = self.w_pin
        wout, b_wout = self.w_pout
        vin = win.rearrange("(k p) f -> p k f", p=128)
        v8 = (lambda sl: sl.rearrange("p (k f) -> p k f", k=8))
        v4 = (lambda sl: sl.rearrange("p (k d) -> p k d", k=4))
        self.wplan(vin[:, :, 512:1024], v8, b_win)
        self.wplan(vin[:, :, 1024:1536], v8, b_win)
        self.wplan(vin[:, :, 1536:2048], v8, b_win)
        self.wplan(vin[:, :, 0:512], v8, b_win)
        self.wplan(wout.rearrange("(k p) d -> p k d", p=128)[:, 0:4, :], v4, b_wout)
        self.wplan(wout.rearrange("(k p) d -> p k d", p=128)[:, 4:8, :], v4, b_wout)

    def mixer1(self):
        nc, P, ps, xs, A = self.nc, self.P, self.ps, self.xs, self.A
        psb = self.psb
        o = (1 * 3 + 1) * 8
        vA = self.vecA[:, o:o + 8]
        vG = self.vecG[:, o:o + 8]
        vS = self.modT[:, 72 + 24:72 + 32]
        sQK = nc.dram_tensor("sQK", [8 * 2 * 64, TOK], BF16, kind="Internal").ap()
        aQK = nc.dram_tensor("aQK", [NCORES * 8 * 2 * 64, TOK], BF16, kind="Internal").ap()
        sV = nc.dram_tensor("sV", [8 * 16 * 128, 64], BF16, kind="Internal").ap()
        aV = nc.dram_tensor("aV", [NCORES * 8 * 16 * 128, 64], BF16, kind="Internal").ap()
        sA_ = nc.dram_tensor("sAtt", [64, SEQ], BF16, kind="Internal").ap()
        aA = nc.dram_tensor("aAtt", [NCORES * 64, SEQ], BF16, kind="Internal").ap()
        b_sQK, b_sV, b_aQK, b_aV, b_sA, b_aA = (Buf("sQK"), Buf("sV"), Buf("aQK"), Buf("aV"), Buf("sA"), Buf("aA"))

        A.push()
        hT = A.bf16(8 * TT).rearrange("p (c t) -> p c t", c=8)
        hb = {t: P.buf("h%d" % t[0]) for t in TILES_H}
        ug, ug_b = A.f32(TT), P.buf("ug")
        HW_ = 1040
        sA, sA_b = A.f32(HW_), P.buf("sA")
        sB, sB_b = A.f32(HW_), P.buf("sB")
        pmx = [(A.bf16(1024), P.buf("pmx%d" % i)) for i in range(2)]
        stg = [(A.bf16(TOK), P.buf("stg%d" % i)) for i in range(2)]
        vst = [(A.bf16(512), P.buf("vst%d" % i)) for i in range(2)]
        pw = A.f32(4 * 128).rearrange("p (g d) -> p g d", g=4)
        pv = A.f32(8).rearrange("p (g v) -> p g v", g=4)
        pc = A.f32(64).rearrange("p (g t) -> p g t", g=4)
        hm = A.f32(64)
        b_pc = P.buf("pconst")
        P.dma("sp", None, pw, self.poolw_d.rearrange("g c d -> c g d"), writes=(b_pc,))
        P.dma("sp", None, pv, self.pvec_d, writes=(b_pc,))
        P.dma("sp", None, pc, self.pcorr_d, writes=(b_pc,))
        P.dma("sp", None, hm, self.hmask_d, writes=(b_pc,))
        for t in TILES_H:
            c0, n = t
            self.norm_tile(t, None, vA, vS, hT[:, :, c0:c0 + n], hb[t])
        sQKv = sQK.rearrange("(h i d) t -> h i d t", h=8, i=2)
        for i_ in range(2):
            w_, wb_ = self.w_get()
            for hc in range(4):
                sg_, sg_b = stg[(i_ * 4 + hc) % 2]
                for bi, t in enumerate(TILES):
                    c0, n = t
                    bank = 1 + bi % 2
                    self.mm([(ps[:, bank, 0:n], w_[:, k, hc * 128:(hc + 1) * 128], hT[:, k, c0:c0 + n], k == 0, k == 7)
                             for k in range(8)], reads=(wb_, hb[t]), writes=(psb[bank],))
                    self.act(sg_[:, c0 - HALO:c0 - HALO + n], ps[:, bank, 0:n], AF.Identity, reads=(psb[bank],),
                             writes=(sg_b,), scale=(0.125 if i_ == 0 else 1.0))
                for hh in range(2):
                    P.dma("sp", None, sQKv[2 * hc + hh, i_], sg_[hh * 64:(hh + 1) * 64, :], reads=(sg_b,),
                          writes=(b_sQK,))
            self.w_release(1)
        wv_, wvb = self.w_get()
        sVv = sV.rearrange("(h ts p) d -> ts p h d", h=8, ts=16)
        for ts in range(16):
            c0 = HALO + ts * 128
            t = TILES[ts // 4]
            vs_, vs_b = vst[ts % 2]
            bank = 1 + ts % 2
            self.mm([(ps[:, bank, 0:512], hT[:, k, c0:c0 + 128], wv_[:, k, :], k == 0, k == 7) for k in range(8)],
                    reads=(wvb, hb[t]), writes=(psb[bank],))
            self.act(vs_, ps[:, bank, 0:512], AF.Identity, reads=(psb[bank],), writes=(vs_b,))
            P.dma("sp", None, sVv[ts], vs_.rearrange("p (h d) -> p h d", h=8), reads=(vs_b,), writes=(b_sV,))
        self.w_release(1)
        s_cc = P.new_sem("ccx")
        P.custom("pool", s_cc, 1,
                 lambda g: g.collective_compute("AllGather", ALU.bypass, replica_groups=[list(range(NCORES))],
                                                ins=[sQK[:, :]], outs=[aQK[:, :]]),
                 reads=(b_sQK,), writes=(b_aQK,))
        P.custom("pool", s_cc, 1,
                 lambda g: g.collective_compute("AllGather", ALU.bypass, replica_groups=[list(range(NCORES))],
                                                ins=[sV[:, :]], outs=[aV[:, :]]),
                 reads=(b_sV,), writes=(b_aV,))

        wu_, wub = self.w_get()
        wo1, wo1b = self.w_get()
        ycnt = 0
        for g_ in range(4):
            win_ = (2, 4, 8, 16)[g_]
            for bi, t in enumerate(TILES_H):
                c0, n = t
                bank = 1 + bi % 2
                self.mm([(ps[:, bank, 0:n], wu_[:, k, g_ * 128:(g_ + 1) * 128], hT[:, k, c0:c0 + n], k == 0, k == 7)
                         for k in range(8)], reads=(wub, hb[t]), writes=(psb[bank],))
                self.act(ug[:, c0:c0 + n], ps[:, bank, 0:n], AF.Identity, reads=(psb[bank],), writes=(ug_b,))
            P.op("dve", lambda g: g.tensor_tensor(ug[:, 0:64], ug[:, 0:64], hm, ALU.mult),
                 reads=(b_pc,), writes=(ug_b,))
            for hp in range(2):
                b0 = 48 + hp * 1024
                src, src_b = None, None
                bufs = [(sA, sA_b), (sB, sB_b)]
                step = 1
                cur = None
                ki = 0
                while step < win_:
                    dst, dst_b = bufs[ki % 2]
                    if cur is None:
                        P.op("dve", lambda g, dst=dst, b0=b0: g.tensor_tensor(
                            dst[:, 1:HW_], ug[:, b0 + 1:b0 + HW_], ug[:, b0:b0 + HW_ - 1], ALU.add),
                            reads=(ug_b,), writes=(dst_b,))
                    else:
                        c_, c_b = cur
                        P.op("dve", lambda g, dst=dst, c_=c_, step=step: g.tensor_tensor(
                            dst[:, step:HW_], c_[:, step:HW_], c_[:, 0:HW_ - step], ALU.add),
                            reads=(c_b,), writes=(dst_b,))
                    cur = (dst, dst_b)
                    step *= 2
                    ki += 1
                c_, c_b = cur
                dst, dst_b = bufs[ki % 2]
                if hp == 0:
                    P.op("dve", lambda g, c_=c_, g_=g_: g.tensor_tensor(
                        c_[:, 16:32], c_[:, 16:32], pc[:, g_, :], ALU.mult), reads=(b_pc,), writes=(c_b,))
                P.op("dve", lambda g, dst=dst, c_=c_, b0=b0, win_=win_: g.scalar_tensor_tensor(
                    out=dst[:, 16:HW_], in0=c_[:, 16:HW_], scalar=1.0 / win_, in1=ug[:, b0 + 16:b0 + HW_],
                    op0=ALU.mult, op1=ALU.subtract), reads=(c_b, ug_b), writes=(dst_b,))
                pm, pm_b = pmx[(g_ * 2 + hp) % 2]
                for ti in range(2):
                    bank = 3 + ti
                    self.mm([(ps[:, bank, 0:512], pw[:, g_, :], dst[:, 16 + ti * 512:16 + (ti + 1) * 512], True, True)],
                            reads=(dst_b, b_pc), writes=(psb[bank],))
                    P.op("dve", lambda g, pm=pm, ti=ti, bank=bank, g_=g_: g.tensor_scalar(
                        pm[:, ti * 512:(ti + 1) * 512], ps[:, bank, 0:512], pv[:, g_, 0:1], pv[:, g_, 1:2],
                        ALU.add, ALU.mult), reads=(psb[bank], b_pc), writes=(pm_b,))
                for ti in range(2):
                    t = TILES[hp * 2 + ti]
                    c0, n = t
                    for dc in range(8):
                        yb = 5 + (ycnt % 2)
                        ycnt += 1
                        self.mm([(ps[:, yb, 0:n], wo1[:, g_, dc * 128:(dc + 1) * 128], pm[:, ti * 512:(ti + 1) * 512],
                                  True, True)], reads=(pm_b, wo1b), writes=(psb[yb],))
                        P.op("dve", lambda g, yb=yb, dc=dc, c0=c0, n=n: g.scalar_tensor_tensor(
                            out=xs[:, dc, c0:c0 + n], in0=ps[:, yb, 0:n], scalar=vG[:, dc:dc + 1],
                            in1=xs[:, dc, c0:c0 + n], op0=ALU.mult, op1=ALU.add),
                            reads=(psb[yb], self.b_mod), writes=(self.xb[t],))
        self.w_release(2)
        A.pop()
        if self.stop_after == "l1a":
            return
        A.push()
        NB = 64
        Kaug = A.bf16(SEQ)
        V_sb = A.bf16(128 * 65).rearrange("p (k d) -> p k d", d=65)
        QA = [(A.bf16(512), P.buf("QA%d" % i), P.buf("QAm%d" % i)) for i in range(2)]
        QB = [(A.bf16(512), P.buf("QB%d" % i), P.buf("QBm%d" % i)) for i in range(2)]
        pt = [(A.bf16(1024).rearrange("p (j t) -> p j t", j=2), P.buf("pt%d" % i)) for i in range(2)]
        km = A.f32(64)
        kmh = A.bf16(64)
        kml = A.bf16(64)
        kab = A.f32(8)
        kabh = A.bf16(8)
        ac = A.f32(4)
        drev = A.f32(127)
        tri = A.bf16(512).rearrange("p (s q) -> p s q", s=2)
        ident = A.bf16(128)
        onesf = A.f32(64)
        gsb = [(A.f32(64), P.buf("gsb%d" % i)) for i in range(2)]
        Mf = [(A.f32(64), P.buf("Mf%d" % i)) for i in range(2)]
        Mh = [(A.bf16(256), P.buf("Mh%d" % i)) for i in range(4)]
        m8 = [(A.f32(8), P.buf("m8%d" % i)) for i in range(2)]
        bq = [(A.f32(8), P.buf("bq%d" % i)) for i in range(2)]
        absq = [(A.bf16(128), P.buf("absq%d" % i)) for i in range(2)]
        osb = [(A.f32(512), P.buf("osb%d" % i)) for i in range(2)]
        rd = [(A.f32(512), P.buf("rd%d" % i)) for i in range(1)] * 2
        ast = [(A.bf16(512), P.buf("ast%d" % i)) for i in range(2)]
        rdh = [(A.bf16(1024), P.buf("rdh%d" % i)) for i in range(1)] * 2
        b_K, b_V, b_ac, b_km = P.buf("K"), P.buf("V"), P.buf("ac"), P.buf("km")

        rank_cache = {}

        def rank_of(g):
            if "r" not in rank_cache:
                rank_cache["r"] = g.partition_id() % NCORES
            return rank_cache["r"]

        aQKv = aQK.rearrange("(r x d) t -> d r x t", r=NCORES, x=16)
        myQ = nc.dram_tensor("myQ", [64, SEQ], BF16, kind="Internal").ap()
        myV = nc.dram_tensor("myV", [SEQ, 64], BF16, kind="Internal").ap()
        b_myQ, b_myV = Buf("myQ"), Buf("myV")
        P.custom("sp", None, 16,
                 lambda g: g.dma_start(out=Kaug[0:64, :].rearrange("p (r t) -> p r t", r=NCORES),
                                       in_=aQKv[:, :, bass.ds(rank_of(g) * 2 + 1, 1), :].rearrange("d r x t -> d (r x) t")),
                 reads=(b_aQK,), writes=(b_K,))
        P.custom("sp", None, 16,
                 lambda g: g.dma_start(out=myQ.rearrange("d (r t) -> d r t", r=NCORES),
                                       in_=aQKv[:, :, bass.ds(rank_of(g) * 2, 1), :].rearrange("d r x t -> d (r x) t")),
                 reads=(b_aQK,), writes=(b_myQ,))
        P.dma("sp", None, Kaug[64:128, :], self.khot_d, writes=(b_K,))
        aVv = aV.rearrange("(r h x) d -> r h x d", r=NCORES, h=8)
        P.custom("sp", None, 16,
                 lambda g: g.dma_start(out=myV.rearrange("(r x) d -> r x d", r=NCORES),
                                       in_=aVv[:, bass.ds(rank_of(g), 1), :, :].rearrange("r h x d -> r (h x) d")),
                 reads=(b_aV,), writes=(b_myV,))
        myVv = myV.rearrange("(r ts p) d -> r p ts d", r=NCORES, ts=16)
        for r in range(NCORES):
            for hf in range(2):
                P.dma("sp", None, V_sb[:, r * 16 + hf * 8:r * 16 + hf * 8 + 8, 0:64], myVv[r][:, hf * 8:hf * 8 + 8, :],
                      reads=(b_myV,), writes=(b_V,))
        P.op("pool", lambda g: g.memset(V_sb[:, :, 64:65], 1.0), writes=(b_V,))
        P.dma("sp", None, ac, self.aconst_d, writes=(b_ac,))
        P.dma("sp", None, drev, self.drev_d, writes=(b_ac,))
        P.dma("sp", None, tri.rearrange("p s q -> p (s q)"), self.tri_d, writes=(b_ac,))
        P.dma("sp", None, ident, self.ident_d, writes=(b_ac,))
        P.op("pool", lambda g: g.memset(onesf, 1.0), writes=(b_ac,))
        for i in range(4):
            P.op("pool", lambda g, i=i: g.memset(Mh[i][0], 0.0), writes=(Mh[i][1],))
        P.op("dve", lambda g: g.tensor_reduce(km[0:64, :], Kaug[0:64, :].rearrange("p (n k) -> p n k", k=256), AX.X, ALU.add),
             reads=(b_K,), writes=(b_km,))
        P.op("dve", lambda g: g.tensor_scalar(km[0:64, :], km[0:64, :], 1.0 / 256, None, ALU.mult),
             reads=(), writes=(b_km,))
        P.op("dve", lambda g: g.tensor_copy(kmh[0:64, :], km[0:64, :]), reads=(), writes=(b_km,))
        P.op("dve", lambda g: g.tensor_tensor(kml[0:64, :], km[0:64, :], kmh[0:64, :], ALU.subtract),
             reads=(), writes=(b_km,))
        P.op("dve", lambda g: g.tensor_reduce(kab[0:64, 0:1], Kaug[0:64, :], AX.X, ALU.max, apply_absolute_value=True),
             reads=(b_K,), writes=(b_km,))
        P.op("dve", lambda g: g.tensor_copy(kabh[0:64, 0:1], kab[0:64, 0:1]), reads=(), writes=(b_km,))

        psT = self.psT
        NQC = SEQ // 512

        def gating_a(qc):
            sl = qc % 2
            qa, qa_b, qam_b = QA[sl]
            qb, qb_b, qbm_b = QB[sl]
            r, lc = qc // 4, (qc % 4) * 512
            useB = (2 * qc + 1) >= 32
            P.dma("sp", None, qa[0:64, :], myQ[:, qc * 512:(qc + 1) * 512], reads=(b_myQ,), writes=(qa_b,))
            if useB:
                P.op("pool", lambda g: g.tensor_copy(qb[0:64, :], qa[0:64, :]), reads=(qa_b,), writes=(qb_b,))
            for tq in range(4):
                qt = qc * 4 + tq
                own = qt // 2
                par = qt % 2
                s2 = qt % 2
                g_, g_b = gsb[s2]
                mf, mf_b = Mf[s2]
                mh, mh_b = Mh[qt % 4]
                m8_, m8_b = m8[s2]
                bq_, bq_b = bq[s2]
                aq_, aq_b = absq[s2]
                qcols = slice(tq * 128, (tq + 1) * 128)
                self.mm([(ps[:, 0, 0:NB], qa[0:64, qcols], kmh[0:64, :], True, False),
                         (ps[:, 0, 0:NB], qa[0:64, qcols], kml[0:64, :], False, True)],
                        reads=(qa_b, b_km), writes=(psb[0],))
                self.act(aq_[0:64, :], qa[0:64, qcols], AF.Abs, reads=(qa_b,), writes=(aq_b,))
                self.mm([(ps[:, 0, 64:65], aq_[0:64, :], kabh[0:64, 0:1], True, True)],
                        reads=(aq_b, b_km), writes=(psb[0],))
                P.op("pool", lambda g, g_=g_: g.memset(g_, NEG), writes=(g_b,))
                P.op("pool", lambda g, mf=mf: g.memset(mf, 0.0), writes=(mf_b,))
                if own > 0:
                    P.op("dve", lambda g, g_=g_, own=own: g.tensor_copy(g_[:, 0:own], ps[:, 0, 0:own]),
                         reads=(psb[0],), writes=(g_b,))
                    P.op("dve", lambda g, g_=g_, m8_=m8_, own=own: g.max(m8_, g_[:, 0:max(own, 8)]),
                         reads=(g_b,), writes=(m8_b,))
                    P.op("dve", lambda g, g_=g_, mf=mf, m8_=m8_, own=own: g.tensor_scalar(
                        mf[:, 0:own], g_[:, 0:own], m8_[:, 2:3], NEG, ALU.is_lt, ALU.mult),
                        reads=(g_b, m8_b), writes=(mf_b,))
                P.op("dve", lambda g, bq_=bq_, par=par: g.tensor_tensor(
                    bq_[:, 0:1], ac[:, par:par + 1], ps[:, 0, 64:65], ALU.subtract),
                    reads=(psb[0], b_ac), writes=(bq_b,))
                P.op("dve", lambda g, mf=mf, bq_=bq_, own=own: g.scalar_tensor_tensor(
                    out=mf, in0=mf, scalar=bq_[:, 0:1], in1=drev[:, 63 - own:127 - own], op0=ALU.add, op1=ALU.add),
                    reads=(bq_b, b_ac), writes=(mf_b,))
                for half in range(2):
                    if half == 1 and not useB:
                        continue
                    cb = half * 128
                    P.op("dve", lambda g, mh=mh, mf=mf, cb=cb, half=half: g.tensor_copy(
                        mh[:, cb + 64:cb + 96], mf[:, half * 32:half * 32 + 32]), reads=(mf_b,), writes=(mh_b,))
                    P.op("dve", lambda g, mh=mh, mf=mf, cb=cb, half=half: g.tensor_tensor(
                        mh[:, cb + 96:cb + 128], mf[:, half * 32:half * 32 + 32], mh[:, cb + 64:cb + 96],
                        ALU.subtract), reads=(mf_b,), writes=(mh_b,))

        def gating_b(qc):
            sl = qc % 2
            qa, qa_b, qam_b = QA[sl]
            qb, qb_b, qbm_b = QB[sl]
            useB = (2 * qc + 1) >= 32
            for tq in range(4):
                qt = qc * 4 + tq
                mh, mh_b = Mh[qt % 4]
                qcols = slice(tq * 128, (tq + 1) * 128)
                for half, (qx, qxm_b) in enumerate(((qa, qam_b), (qb, qbm_b))):
                    if half == 1 and not useB:
                        continue
                    cb = half * 128
                    tcol = ((qt * 2 + half) % 8) * 128
                    P.op("pe", lambda g, mh=mh, cb=cb, tcol=tcol: g.transpose(
                        psT[:, tcol:tcol + 128], mh[:, cb:cb + 128], ident), reads=(mh_b, b_ac), writes=(self.psTb,))
                    self.P.op("act", lambda g, qx=qx, qcols=qcols, tcol=tcol: g.activation(
                        out=qx[64:128, qcols], in_=psT[64:128, tcol:tcol + 128], func=AF.Identity),
                        reads=(self.psTb,), writes=(qxm_b,))

        def attend(qc):
            sl = qc % 2
            qa, qa_b, qam_b = QA[sl]
            qb, qb_b, qbm_b = QB[sl]
            b0 = 2 * qc
            nkb = 4 * qc + 4
            ob = 5 + qc % 2
            def S_(kb, sb_):
                n_ = kb // 2
                if n_ < 32:
                    qx, rds = qa, (qa_b, qam_b)
                else:
                    qx, rds = qb, (qb_b, qbm_b)
                mms = [(ps[:, sb_, 0:512], Kaug[:, kb * 128:(kb + 1) * 128], qx[:, :], True, not (n_ >= b0))]
                if n_ == b0:
                    mms.append((ps[:, sb_, 0:256], ident, tri[:, kb % 2, :], False, True))
                elif n_ == b0 + 1:
                    mms.append((ps[:, sb_, 256:512], ident, tri[:, kb % 2, :], False, True))
                self.mm(mms, reads=rds + (b_K, b_ac), writes=(psb[sb_],))

            pairs = []
            for m in range(nkb // 4):
                pairs.append((4 * m, 4 * m + 2))
                pairs.append((4 * m + 1, 4 * m + 3))

            def SP(pi):
                base = 1 + 2 * (pi % 2)
                S_(pairs[pi][0], base)
                S_(pairs[pi][1], base + 1)

            SP(0)
            for pi, (k0, k1) in enumerate(pairs):
                base = 1 + 2 * (pi % 2)
                p_, p_b = pt[pi % 2]
                self.act(p_, ps[:, base:base + 2, :], AF.Exp, reads=(psb[base], psb[base + 1], b_ac), writes=(p_b,),
                         bias=ac[:, 2 + k0 % 2:3 + k0 % 2])
                if pi + 1 < len(pairs):
                    SP(pi + 1)
                for j_, kb in enumerate((k0, k1)):
                    self.mm([(ps[0:65, ob, 0:512], V_sb[:, kb, :], p_[:, j_, :], kb == 0, kb == nkb - 1)],
                            reads=(p_b, b_V), writes=(psb[ob],))
                if pi == 1 and qc + 1 < NQC:
                    gating_a(qc + 1)

        def finalize(qc):
            ob = 5 + qc % 2
            o_, o_b = osb[qc % 2]
            r_, r_b = rd[qc % 2]
            a_, a_b = ast[qc % 2]
            P.op("dve", lambda g: g.tensor_copy(o_[0:65, :], ps[0:65, ob, 0:512]), reads=(psb[ob],), writes=(o_b,))
            rh_, rh_b = rdh[qc % 2]
            P.op("dve", lambda g: g.reciprocal(r_[64:65, :], o_[64:65, :]), reads=(o_b,), writes=(r_b,))
            P.op("dve", lambda g: g.tensor_copy(rh_[64:65, 0:512], r_[64:65, :]), reads=(r_b,), writes=(rh_b,))
            P.op("dve", lambda g: g.tensor_tensor(rh_[64:65, 512:1024], r_[64:65, :], rh_[64:65, 0:512], ALU.subtract),
                 reads=(r_b,), writes=(rh_b,))
            self.mm([(ps[0:64, 0, 0:512], self.ones_bf[64:65, 0:64], rh_[64:65, 0:512], True, False),
                     (ps[0:64, 0, 0:512], self.ones_bf[64:65, 0:64], rh_[64:65, 512:1024], False, True)],
                    reads=(rh_b, self.b_const), writes=(psb[0],))
            P.op("dve", lambda g: g.tensor_tensor(a_[0:64, :], o_[0:64, :], ps[0:64, 0, 0:512], ALU.mult),
                 reads=(o_b, psb[0]), writes=(a_b,))
            P.dma("sp", None, sA_[:, qc * 512:(qc + 1) * 512], a_[0:64, :], reads=(a_b,), writes=(b_sA,))

        gating_a(0)
        gating_b(0)
        for qc in range(NQC):
            attend(qc)
            if qc + 1 < NQC:
                gating_b(qc + 1)
            finalize(qc)
        A.pop()
        P.custom("pool", s_cc, 1,
                 lambda g: g.collective_compute("AllGather", ALU.bypass, replica_groups=[list(range(NCORES))],
                                                ins=[sA_[:, :]], outs=[aA[:, :]]),
                 reads=(b_sA,), writes=(b_aA,))

        A.push()
        mix2 = A.bf16(4 * TOK).rearrange("p (c t) -> p c t", c=4)
        b_m2 = P.buf("mix2")
        aAv = aA.rearrange("(c p) t -> p c t", p=128)
        P.custom("sp", None, 16,
                 lambda g: g.dma_start(out=mix2, in_=aAv[:, :, bass.ds(rank_of(g) * TOK, TOK)]),
                 reads=(b_aA,), writes=(b_m2,))
        wo2, wo2b = self.w_get()
        ycnt = 0
        for t in TILES:
            c0, n = t
            for dc in range(8):
                yb = 5 + (ycnt % 2)
                ycnt += 1
                self.mm([(ps[:, yb, 0:n], wo2[:, kc, dc * 128:(dc + 1) * 128], mix2[:, kc, c0 - HALO:c0 - HALO + n],
                          kc == 0, kc == 3) for kc in range(4)], reads=(b_m2, wo2b), writes=(psb[yb],))
                P.op("dve", lambda g, yb=yb, dc=dc, c0=c0, n=n: g.scalar_tensor_tensor(
                    out=xs[:, dc, c0:c0 + n], in0=ps[:, yb, 0:n], scalar=vG[:, dc:dc + 1],
                    in1=xs[:, dc, c0:c0 + n], op0=ALU.mult, op1=ALU.add),
                    reads=(psb[yb], self.b_mod), writes=(self.xb[t],))
        self.w_release(1)
        A.pop()

    def plan_ffn(self, wg, wu, wd):
        for f0 in range(0, NFC, 4):
            nf = min(4, NFC - f0)
            vgu = (lambda sl, nf=nf: sl[:, 0:8 * nf * 128].rearrange("p (k f) -> p k f", k=8))
            vd = (lambda sl, nf=nf: sl[:, 0:nf * 1024].rearrange("p (f d) -> p f d", f=nf))
            self.wplan(wg.rearrange("(k p) f -> p k f", p=128)[:, :, f0 * 128:(f0 + nf) * 128], vgu, self.wdeps[0])
            self.wplan(wu.rearrange("(k p) f -> p k f", p=128)[:, :, f0 * 128:(f0 + nf) * 128], vgu, self.wdeps[1])
            self.wplan(wd.rearrange("(f p) d -> p f d", p=128)[:, f0:f0 + nf, :], vd, self.wdeps[2])

    def ffn(self, L, s, tiles):
        P, ps, xs, A = self.P, self.ps, self.xs, self.A
        o = (L * 3 + s) * 8
        vA = self.vecA[:, o:o + 8]
        vG = self.vecG[:, o:o + 8]
        vS = self.modT[:, L * 72 + s * 24:L * 72 + s * 24 + 8]
        A.push()
        hT = A.bf16(8 * TT).rearrange("p (c t) -> p c t", c=8)
        hb = {t: P.buf("h%d" % t[0]) for t in tiles}
        asl = [A.bf16(4 * 512).rearrange("p (f t) -> p f t", f=4) for _ in range(2)]
        ab_ = [P.buf("a0"), P.buf("a1")]
        sg = [A.f32(512) for _ in range(2)]
        sgb = [P.buf("sg0"), P.buf("sg1")]
        for t in tiles:
            c0, n = t
            self.norm_tile(t, None, vA, vS, hT[:, :, c0:c0 + n], hb[t])
        groups = [(f0, min(4, NFC - f0)) for f0 in range(0, NFC, 4)]
        steps = [(gi, t) for gi in range(len(groups)) for t in tiles]
        W = {}
        cnt = {"fi": 0, "y": 0}

        def GU(i):
            gi, t = steps[i]
            if gi not in W:
                W[gi] = (self.w_get(), self.w_get(), self.w_get())
            (wgv, wgb), (wuv, wub), _ = W[gi]
            nf = groups[gi][1]
            c0, n = t
            a_t, a_b = asl[i % 2], ab_[i % 2]
            for fi in range(nf):
                k2 = cnt["fi"] % 2
                cnt["fi"] += 1
                gi_, ui = 1 + k2, 3 + k2
                self.mm([(ps[:, gi_, 0:n], wgv[:, k, fi * 128:(fi + 1) * 128], hT[:, k, c0:c0 + n], k == 0, k == 7)
                         for k in range(8)], reads=(wgb, hb[t]), writes=(self.psb[gi_],))
                self.mm([(ps[:, ui, 0:n], wuv[:, k, fi * 128:(fi + 1) * 128], hT[:, k, c0:c0 + n], k == 0, k == 7)
                         for k in range(8)], reads=(wub, hb[t]), writes=(self.psb[ui],))
                self.act(sg[k2][:, 0:n], ps[:, gi_, 0:n], AF.Silu, reads=(self.psb[gi_],), writes=(sgb[k2],))
                P.op("dve", lambda g, k2=k2, ui=ui, fi=fi, a_t=a_t, n=n: g.tensor_tensor(
                    a_t[:, fi, 0:n], sg[k2][:, 0:n], ps[:, ui, 0:n], ALU.mult),
                    reads=(sgb[k2], self.psb[ui]), writes=(a_b,))

        def DN(i):
            gi, t = steps[i]
            _, _, (wdv, wdb) = W[gi]
            nf = groups[gi][1]
            c0, n = t
            a_t, a_b = asl[i % 2], ab_[i % 2]
            for dc in range(8):
                yb = 5 + (cnt["y"] % 2)
                cnt["y"] += 1
                self.mm([(ps[:, yb, 0:n], wdv[:, fi, dc * 128:(dc + 1) * 128], a_t[:, fi, 0:n], fi == 0, fi == nf - 1)
                         for fi in range(nf)], reads=(wdb, a_b), writes=(self.psb[yb],))
                P.op("dve", lambda g, yb=yb, dc=dc, c0=c0, n=n: g.scalar_tensor_tensor(
                    out=xs[:, dc, c0:c0 + n], in0=ps[:, yb, 0:n], scalar=vG[:, dc:dc + 1],
                    in1=xs[:, dc, c0:c0 + n], op0=ALU.mult, op1=ALU.add),
                    reads=(self.psb[yb], self.b_mod), writes=(self.xb[t],))
            if i + 1 == len(steps) or steps[i + 1][0] != gi:
                self.w_release(3)

        GU(0)
        for i in range(len(steps)):
            if i + 1 < len(steps):
                GU(i + 1)
            DN(i)
        A.pop()


_CACHE = {}
GATHER = False


def _prep_inputs(inp, stop_after=None):
    x = np.asarray(inp["x"], dtype=np.float32)[0]
    c = np.asarray(inp["c"], dtype=np.float32)[0]
    ada_w = np.asarray(inp["ada_w"], dtype=np.float32)
    ada_b = np.asarray(inp["ada_b"], dtype=np.float32)
    ffn_norm = np.asarray(inp["ffn_norm"], dtype=np.float32)
    mix_norm = np.asarray(inp["mix_norm"], dtype=np.float32)
    final_norm = np.asarray(inp["final_norm"], dtype=np.float32)
    xT = np.ascontiguousarray(x.T)
    xTp = np.concatenate([np.zeros((D, HALO), np.float32), xT], axis=1)
    adaw_chunks = ada_w.reshape(2, D, 72, 128).transpose(0, 2, 1, 3).reshape(144, D, 128)
    adab_chunks = ada_b.reshape(144, 128)
    gains = np.stack([ffn_norm[0, 0], mix_norm[0], ffn_norm[0, 1], ffn_norm[1, 0], mix_norm[1],
                      ffn_norm[1, 1], final_norm])
    normg = np.ascontiguousarray(gains.reshape(7, 8, 128).transpose(2, 0, 1))
    shared = {
        "cT": np.ascontiguousarray(c.reshape(8, 128).T),
        "normg": normg,
    }
    caw = np.asarray(inp["conv_a_w"], dtype=np.float32)[0]
    cvec = np.concatenate([
        caw.reshape(31, 4, 128).transpose(2, 1, 0),
        np.asarray(inp["conv_a_b"], dtype=np.float32)[0].reshape(4, 128).T[:, :, None],
        np.asarray(inp["conv_a_ln_g"], dtype=np.float32)[0].reshape(4, 128).T[:, :, None],
        np.asarray(inp["conv_a_ln_b"], dtype=np.float32)[0].reshape(4, 128).T[:, :, None],
        np.asarray(inp["conv_b_w"], dtype=np.float32)[0].reshape(3, 4, 128).transpose(2, 1, 0),
    ], axis=2)
    shared["cvec"] = np.ascontiguousarray(cvec.reshape(128, 4 * 37))
    import ml_dtypes
    bf = ml_dtypes.bfloat16
    shared["pool_w"] = np.ascontiguousarray(np.asarray(inp["pool_w"], dtype=np.float32)[0])
    pb = np.asarray(inp["pool_b"], dtype=np.float32)[0]
    psc = np.asarray(inp["pool_scale"], dtype=np.float32)[0].reshape(4, 128)
    shared["pvec"] = np.ascontiguousarray(np.stack([pb.T, psc.T], axis=2).reshape(128, 8))
    blk = np.arange(SEQ) // 256
    khot = np.zeros((64, SEQ), np.float32)
    khot[blk % 32, np.arange(SEQ)] = 1.0
    khot[32 + blk % 32, np.arange(SEQ)] = 1.0
    shared["khot"] = khot.astype(bf)
    kk = np.arange(128)[:, None, None] + 128 * np.arange(2)[None, :, None]
    qq = np.arange(256)[None, None, :]
    shared["tri"] = np.where(kk > qq, NEG, 0.0).astype(np.float32).reshape(128, 512).astype(bf)
    shared["ident"] = np.eye(128, dtype=np.float32).astype(bf)
    weights = {
        "pm_w_in": np.asarray(inp["pm_w_in"], dtype=np.float32)[0],
        "pm_w_out": np.asarray(inp["pm_w_out"], dtype=np.float32)[0],
        "conv_w_in": np.asarray(inp["conv_w_in"], dtype=np.float32)[0],
        "conv_w_out": np.asarray(inp["conv_w_out"], dtype=np.float32)[0],
        "ffn_wg": np.asarray(inp["ffn_w_gate"], dtype=np.float32).reshape(4 * D, DFF),
        "ffn_wu": np.asarray(inp["ffn_w_up"], dtype=np.float32).reshape(4 * D, DFF),
        "ffn_wd": np.asarray(inp["ffn_w_down"], dtype=np.float32).reshape(4 * DFF, D),
    }
    maps = []
    for r in range(NCORES):
        m = dict(shared)
        m["xT"] = np.ascontiguousarray(xTp[:, r * TOK:r * TOK + TT])
        m["adaw"] = np.ascontiguousarray(adaw_chunks[r * 18:(r + 1) * 18])
        m["adab"] = np.ascontiguousarray(adab_chunks[r * 18:(r + 1) * 18].T)
        m["hmask"] = np.full((128, 64), 0.0 if r == 0 else 1.0, np.float32)
        pcorr = np.ones((4, 16), np.float32)
        if r == 0:
            for gi, w_ in enumerate((2, 4, 8, 16)):
                pcorr[gi] = w_ / np.minimum(np.arange(16) + 1, w_)
        m["pcorr"] = np.ascontiguousarray(np.broadcast_to(pcorr.reshape(1, 64), (128, 64)))
        slope = 2.0 ** (-(r + 1))
        pp = np.arange(128, dtype=np.float64)
        m["aconst"] = np.stack([-slope * pp, -slope * (128 + pp), slope * pp, slope * (128 + pp)], axis=1).astype(np.float32)
        ii = np.arange(127)
        dr = np.where(ii <= 63, -slope * 256.0 * (63 - ii), NEG).astype(np.float32)
        m["drev"] = np.ascontiguousarray(np.broadcast_to(dr[None, :], (128, 127)))
        for k, w in weights.items():
            if GATHER:
                n = w.shape[0] // NCORES
                m[k] = np.ascontiguousarray(w[r * n:(r + 1) * n])
            else:
                m[k] = w
        maps.append(m)
    return maps


def kernel(stop_after=None, **inp):
    key = (stop_after, GATHER)
    if key not in _CACHE:
        _CACHE[key] = Builder(stop_after, GATHER).build()
    nc = _CACHE[key]
    maps = _prep_inputs(inp, stop_after)
    res = run_bass_kernel_spmd(nc, maps, core_ids=list(range(NCORES)))
    outT = np.concatenate([np.asarray(r["outT"]) for r in res.results], axis=1)
    return np.ascontiguousarray(outT.T)[None].astype(np.float32)
```
